# Optimizing a Trainium2 kernel written in Bass

```python
import jax, jax.numpy as jnp
from jax import lax
import numpy as np

D_MODEL = 1024
BATCH = 16
SEQ = 4096
DEPTH = 4

D_SGU = 1024
SGU_CHUNK = 128
SGU_GROUPS = 8
SGU_GROUP_DIM = D_SGU // SGU_GROUPS
D_RNN = 1024
RG_HEADS = 8
RG_HEAD_DIM = D_RNN // RG_HEADS
CONV_WIDTH = 4
RG_C = 8.0
IN_WIDTH = 2 * D_SGU + 2 * D_RNN + 2 * D_MODEL
IN_SPLITS = (D_SGU, 2 * D_SGU, 2 * D_SGU + D_RNN, 2 * D_SGU + 2 * D_RNN, 2 * D_SGU + 2 * D_RNN + D_MODEL)
N_EXPERTS = 64
TOP_K = 8
N_GROUPS = 8
TOPK_GROUPS = 4
D_EXPERT = 256
D_SHARED = 256
ROUTED_SCALE = 2.5
MOE_BLOCK = 128
DEEPNORM_ALPHA = (2 * DEPTH) ** 0.25
DEEPNORM_BETA = (8 * DEPTH) ** -0.25
LN_EPS = 1e-5

kernel_name = 'hybrid_sgu_rglru_moe_deepnorm_adaln'


def _layer_norm(x, g, b):
    xf = x.astype(jnp.float32)
    mu = jnp.mean(xf, axis=-1, keepdims=True)
    var = jnp.mean(jnp.square(xf - mu), axis=-1, keepdims=True)
    return ((xf - mu) * lax.rsqrt(var + LN_EPS)).astype(x.dtype) * g + b


def _chunk_spatial(v, w_s, b_s):
    B, S, C = v.shape
    vc = v.reshape(B, S // SGU_CHUNK, SGU_CHUNK, SGU_GROUPS, SGU_GROUP_DIM)
    w = w_s * jnp.tril(jnp.ones((SGU_CHUNK, SGU_CHUNK), w_s.dtype))
    out = jnp.einsum('gts,bnsgc->bntgc', w, vc) + b_s.T[:, :, None]
    return out.reshape(B, S, C)


def _causal_conv(x, w, b):
    S = x.shape[1]
    xp = jnp.pad(x, ((0, 0), (CONV_WIDTH - 1, 0), (0, 0)))
    return sum(xp[:, k:k + S] * w[k] for k in range(CONV_WIDTH)) + b


def _rg_lru(xb, wa, ba, wx, bx, lam):
    B, S, _ = xb.shape
    xh = xb.reshape(B, S, RG_HEADS, RG_HEAD_DIM)
    r = jax.nn.sigmoid(jnp.einsum('bshi,hij->bshj', xh, wa).reshape(B, S, D_RNN) + ba)
    i = jax.nn.sigmoid(jnp.einsum('bshi,hij->bshj', xh, wx).reshape(B, S, D_RNN) + bx)
    log_a = -RG_C * r.astype(jnp.float32) * jax.nn.softplus(-lam.astype(jnp.float32))
    a = jnp.exp(log_a)
    inp = jnp.sqrt(-jnp.expm1(2.0 * log_a)) * (i * xb).astype(jnp.float32)

    def combine(left, right):
        a1, b1 = left
        a2, b2 = right
        return a1 * a2, a2 * b1 + b2

    _, h = lax.associative_scan(combine, (a, inp), axis=1)
    return h.astype(xb.dtype)


def _route(h, w_r, b_r):
    B, S, _ = h.shape
    scores = jax.nn.sigmoid(jnp.einsum('bsd,de->bse', h.astype(jnp.float32), w_r.astype(jnp.float32)))
    biased = scores + b_r.astype(jnp.float32)
    grouped = biased.reshape(B, S, N_GROUPS, N_EXPERTS // N_GROUPS)
    group_score = lax.top_k(grouped, 2)[0].sum(-1)
    _, top_groups = lax.top_k(group_score, TOPK_GROUPS)
    group_keep = jax.nn.one_hot(top_groups, N_GROUPS, dtype=jnp.float32).sum(-2) > 0
    expert_keep = jnp.repeat(group_keep, N_EXPERTS // N_GROUPS, axis=-1)
    _, idx = lax.top_k(jnp.where(expert_keep, biased, -jnp.inf), TOP_K)
    w = jnp.take_along_axis(scores, idx, axis=-1)
    w = w / jnp.sum(w, axis=-1, keepdims=True) * ROUTED_SCALE
    return idx, w.astype(h.dtype)


def _moe_row(xs, idx, wts, w1, w3, w2):
    T, D = xs.shape
    K = idx.shape[1]
    E = w1.shape[0]
    n_blocks = -(-(T * K) // MOE_BLOCK) + E
    flat_e = idx.reshape(-1)
    order = jnp.argsort(flat_e)
    e_sorted = flat_e[order]
    tok_sorted = order // K
    counts = jnp.bincount(flat_e, length=E)
    padded = (counts + MOE_BLOCK - 1) // MOE_BLOCK * MOE_BLOCK
    pad_end = jnp.cumsum(padded)
    pad_start = pad_end - padded
    grp_start = jnp.cumsum(counts) - counts
    pos = jnp.arange(T * K) - grp_start[e_sorted] + pad_start[e_sorted]
    buf = jnp.zeros((n_blocks * MOE_BLOCK, D), xs.dtype).at[pos].set(xs[tok_sorted])
    block_e = jnp.minimum(jnp.searchsorted(pad_end, jnp.arange(n_blocks) * MOE_BLOCK, side='right'), E - 1)

    def expert_block(args):
        xb, e = args
        hid = jax.nn.silu(xb @ w1[e]) * (xb @ w3[e])
        return hid @ w2[e]

    yb = lax.map(expert_block, (buf.reshape(n_blocks, MOE_BLOCK, D), block_e))
    y = yb.reshape(-1, D)[pos] * wts.reshape(-1)[order][:, None]
    return jax.ops.segment_sum(y, tok_sorted, num_segments=T)


def setup_inputs(seed: int = 0) -> dict:
    key = jax.random.key(seed)
    ks = iter(jax.random.split(key, 40))
    L, D = DEPTH, D_MODEL
    beta = DEEPNORM_BETA

    def nrm(shape, scale):
        return jax.random.normal(next(ks), shape, jnp.float32) * scale

    a0 = jax.random.uniform(next(ks), (L, D_RNN), jnp.float32, minval=0.9, maxval=0.999)
    rg_lambda = jnp.log(a0) - jnp.log1p(-a0)
    return {
        'x': nrm((BATCH, SEQ, D), 1.0),
        'c': nrm((BATCH, D), 1.0),
        'ada_w': nrm((L, D, 6 * D), 0.5 * D ** -0.5),
        'ada_b': nrm((L, 6 * D), 0.02),
        'w_in': nrm((L, D, IN_WIDTH), D ** -0.5),
        'sgu_ln_g': 1.0 + nrm((L, D_SGU), 0.02),
        'sgu_ln_b': nrm((L, D_SGU), 0.02),
        'sgu_w': nrm((L, SGU_GROUPS, SGU_CHUNK, SGU_CHUNK), SGU_CHUNK ** -0.5),
        'sgu_b': 1.0 + nrm((L, SGU_GROUPS, SGU_CHUNK), 0.02),
        'conv_w': nrm((L, CONV_WIDTH, D_RNN), CONV_WIDTH ** -0.5),
        'conv_b': nrm((L, D_RNN), 0.02),
        'rg_wa': nrm((L, RG_HEADS, RG_HEAD_DIM, RG_HEAD_DIM), RG_HEAD_DIM ** -0.5),
        'rg_ba': nrm((L, D_RNN), 0.02),
        'rg_wx': nrm((L, RG_HEADS, RG_HEAD_DIM, RG_HEAD_DIM), RG_HEAD_DIM ** -0.5),
        'rg_bx': nrm((L, D_RNN), 0.02),
        'rg_lambda': rg_lambda,
        'w_branch_a': nrm((L, D_SGU, D), beta * D_SGU ** -0.5),
        'w_branch_b': nrm((L, D_RNN, D), beta * D_RNN ** -0.5),
        'w_out': nrm((L, D, D), beta * D ** -0.5),
        'ln1_g': 1.0 + nrm((L, D), 0.02),
        'ln1_b': nrm((L, D), 0.02),
        'router_w': nrm((L, D, N_EXPERTS), D ** -0.5),
        'router_b': nrm((L, N_EXPERTS), 0.01),
        'exp_w1': nrm((L, N_EXPERTS, D, D_EXPERT), D ** -0.5),
        'exp_w3': nrm((L, N_EXPERTS, D, D_EXPERT), D ** -0.5),
        'exp_w2': nrm((L, N_EXPERTS, D_EXPERT, D), beta * D_EXPERT ** -0.5),
        'sh_w1': nrm((L, D, D_SHARED), D ** -0.5),
        'sh_w3': nrm((L, D, D_SHARED), D ** -0.5),
        'sh_w2': nrm((L, D_SHARED, D), beta * D_SHARED ** -0.5),
        'ln2_g': 1.0 + nrm((L, D), 0.02),
        'ln2_b': nrm((L, D), 0.02),
    }


def reference(x, c, ada_w, ada_b, w_in, sgu_ln_g, sgu_ln_b, sgu_w, sgu_b, conv_w, conv_b,
              rg_wa, rg_ba, rg_wx, rg_bx, rg_lambda, w_branch_a, w_branch_b, w_out, ln1_g, ln1_b,
              router_w, router_b, exp_w1, exp_w3, exp_w2, sh_w1, sh_w3, sh_w2, ln2_g, ln2_b):
    c_act = jax.nn.silu(c)
    for l in range(DEPTH):
        ada = c_act @ ada_w[l] + ada_b[l]
        shift1, scale1, gate1, shift2, scale2, gate2 = [t[:, None, :] for t in jnp.split(ada, 6, axis=-1)]

        h = x * (1.0 + scale1) + shift1
        z = h @ w_in[l]
        u, v, rnn_gate, rnn_in, gate_a, gate_b = jnp.split(z, IN_SPLITS, axis=-1)
        v = _layer_norm(jax.nn.gelu(v), sgu_ln_g[l], sgu_ln_b[l])
        y_a = jax.nn.gelu(u) * _chunk_spatial(v, sgu_w[l], sgu_b[l])
        r_in = _causal_conv(rnn_in, conv_w[l], conv_b[l])
        y_b = jax.nn.gelu(rnn_gate) * _rg_lru(r_in, rg_wa[l], rg_ba[l], rg_wx[l], rg_bx[l], rg_lambda[l])
        merged = jax.nn.sigmoid(gate_a) * (y_a @ w_branch_a[l]) + jax.nn.sigmoid(gate_b) * (y_b @ w_branch_b[l])
        mix = merged @ w_out[l]
        x = _layer_norm(DEEPNORM_ALPHA * x + gate1 * mix, ln1_g[l], ln1_b[l])

        h2 = x * (1.0 + scale2) + shift2
        idx, wts = _route(h2, router_w[l], router_b[l])
        w1, w3, w2 = exp_w1[l], exp_w3[l], exp_w2[l]
        routed = lax.map(lambda a: _moe_row(a[0], a[1], a[2], w1, w3, w2), (h2, idx, wts))
        shared = (jax.nn.silu(h2 @ sh_w1[l]) * (h2 @ sh_w3[l])) @ sh_w2[l]
        x = _layer_norm(DEEPNORM_ALPHA * x + gate2 * (routed + shared), ln2_g[l], ln2_b[l])
    return x
```

```python
import contextlib
import numpy as np
import concourse.bass as bass
import concourse.mybir as mybir
from concourse.bass_utils import run_bass_kernel_spmd

F32 = mybir.dt.float32
BF16 = mybir.dt.bfloat16
AF = mybir.ActivationFunctionType
ALU = mybir.AluOpType
AX = mybir.AxisListType

L_FULL = 4
D = 1024
T = 256
NE = 64
ALPHA = 8.0 ** 0.25
EPS = 1e-5
C_G = 0.7978845608028654
SQ_G = 0.044715 ** 0.5
NSLOT = 4


class Prog:
    ENGS = ("pe", "act", "dve", "pool", "sp")

    def __init__(self):
        self.ops = {e: [] for e in self.ENGS}
        self.res = {}
        self.dma_sem_count = {}

    @staticmethod
    def _canon(names):
        out = []
        for n in names:
            if n.startswith("ps") and n[2:].isdigit():
                n = "pb%d" % (int(n[2:]) // 2)
            if n not in out:
                out.append(n)
        return out

    def op(self, eng, fn, reads=(), writes=(), dma_sem=None, after=()):
        reads = self._canon(reads)
        writes = self._canon(writes)
        idx = len(self.ops[eng])
        me = (eng, idx)
        deps = set(after)
        for r in reads:
            st = self.res.get(r)
            if st is None:
                st = self.res[r] = {"w": None, "r": {}, "rd": []}
            if st["w"] is not None:
                deps.add(st["w"])
        for w in writes:
            st = self.res.get(w)
            if st is None:
                st = self.res[w] = {"w": None, "r": {}, "rd": []}
            if st["w"] is not None:
                deps.add(st["w"])
            for e2, i2 in st["r"].items():
                deps.add((e2, i2))
            for d in st["rd"]:
                deps.add(d)
        if eng == "pe":
            deps = {d for d in deps if d[0] != "pe"}
        deps.discard(me)
        rec = {"fn": fn, "deps": deps, "signal": False, "dma_sem": dma_sem, "tok": None}
        self.ops[eng].append(rec)
        is_dma = dma_sem is not None
        for r in reads:
            st = self.res[r]
            if is_dma:
                st["rd"].append(me)
            else:
                st["r"][eng] = idx
        for w in writes:
            self.res[w] = {"w": me, "r": {}, "rd": []}
        return me

    def finalize(self):
        for e in self.ENGS:
            for rec in self.ops[e]:
                for (de, di) in rec["deps"]:
                    self.ops[de][di]["signal"] = True
        for e in self.ENGS:
            cnt = 0
            for rec in self.ops[e]:
                if rec["dma_sem"] is not None:
                    s = rec["dma_sem"]
                    self.dma_sem_count[s] = self.dma_sem_count.get(s, 0) + 16
                    rec["tok"] = (s, self.dma_sem_count[s])
                    rec["signal"] = True
                elif rec["signal"]:
                    cnt += 1
                    rec["tok"] = (e, cnt)

    def emit(self, eng_name, eng, sems):
        waited = {}
        for rec in self.ops[eng_name]:
            need = {}
            for (de, di) in rec["deps"]:
                s, v = self.ops[de][di]["tok"]
                if need.get(s, 0) < v:
                    need[s] = v
            for s, v in need.items():
                if waited.get(s, 0) >= v:
                    continue
                eng.wait_ge(sems[s], v)
                waited[s] = v
            inst = rec["fn"](eng)
            if rec["signal"]:
                s, v = rec["tok"]
                inst.then_inc(sems[s], 16 if rec["dma_sem"] is not None else 1)


WNAMES = ["ada_w", "ada_b", "w_in", "sgu_ln_g", "sgu_ln_b", "sgu_w", "sgu_b", "conv_w", "conv_b",
          "rg_wa", "rg_ba", "rg_wx", "rg_bx", "rg_lambda", "w_branch_a", "w_branch_b", "w_out",
          "ln1_g", "ln1_b", "router_w", "router_b", "exp_w1", "exp_w3", "exp_w2", "sh_w1", "sh_w3",
          "sh_w2", "ln2_g", "ln2_b"]
WSHAPES = {
    "ada_w": [4, 1024, 6144], "ada_b": [4, 6144], "w_in": [4, 1024, 6144], "sgu_ln_g": [4, 1024],
    "sgu_ln_b": [4, 1024], "sgu_w": [4, 8, 128, 128], "sgu_b": [4, 8, 128], "conv_w": [4, 4, 1024],
    "conv_b": [4, 1024], "rg_wa": [4, 8, 128, 128], "rg_ba": [4, 1024], "rg_wx": [4, 8, 128, 128],
    "rg_bx": [4, 1024], "rg_lambda": [4, 1024], "w_branch_a": [4, 1024, 1024],
    "w_branch_b": [4, 1024, 1024], "w_out": [4, 1024, 1024], "ln1_g": [4, 1024], "ln1_b": [4, 1024],
    "router_w": [4, 1024, 64], "router_b": [4, 64], "exp_w1": [4, 64, 1024, 256],
    "exp_w3": [4, 64, 1024, 256], "exp_w2": [4, 64, 256, 1024], "sh_w1": [4, 1024, 256],
    "sh_w3": [4, 1024, 256], "sh_w2": [4, 256, 1024], "ln2_g": [4, 1024], "ln2_b": [4, 1024],
}
PV = ["sgu_ln_g", "sgu_ln_b", "conv_b", "rg_ba", "rg_bx", "rg_lambda", "ln1_g", "ln1_b", "ln2_g", "ln2_b",
      "cw0", "cw1", "cw2", "cw3", "ab0", "ab1", "ab2", "ab3", "ab4", "ab5"]


def build_program(nseq=2, nt=16, depth=4, seq=4096, dbg=None, phases="ABC", nlayers_cast=None):
    nc = bass.Bass("TRN2", target_bir_lowering=False)
    P = Prog()
    LD = depth
    x_d = nc.dram_tensor("x", [nseq, seq, D], F32, kind="ExternalInput").ap()
    c_d = nc.dram_tensor("c", [nseq, D], F32, kind="ExternalInput").ap()
    W = {n: nc.dram_tensor(n, WSHAPES[n], F32, kind="ExternalInput").ap() for n in WNAMES}
    out_d = nc.dram_tensor("out", [nseq, nt * T, D], F32, kind="ExternalOutput").ap()
    wmix_d = [nc.dram_tensor("wmix%d" % l, [18, 128, 4096], BF16, kind="Internal").ap() for l in range(LD)]
    wexp_d = [nc.dram_tensor("wexp%d" % l, [NE + 1, 128, 6144], BF16, kind="Internal").ap() for l in range(LD)]
    dbg_d = {}
    if dbg:
        for name, shape in dbg.items():
            dbg_d[name] = nc.dram_tensor("dbg_" + name, shape, F32, kind="ExternalOutput").ap()

    es = contextlib.ExitStack()
    with es:
        def sb(name, shape, dt=F32):
            return es.enter_context(nc.sbuf_tensor(name, shape, dt))

        ident = sb("ident", [128, 128]); onesD = sb("onesD", [128, 128]); onesb = sb("onesb", [128, 128], BF16)
        identb = sb("identb", [128, 128], BF16); tril = sb("tril", [128, 128])
        stgp = sb("stgp", [128, 6, 128]); prm = sb("prm", [128, 6, 128])
        adaS = sb("adaS", [128, LD, 6, 8, nseq]); dv = sb("dv", [128, LD, nseq, 6, 8])
        cact = sb("cact", [128, 16])
        spm = sb("spm", [128, LD, 4, 8])
        WsT = sb("WsT", [128, LD, 8, 128], BF16); bias2 = sb("bias2", [128, LD, 8, 128])
        rgw = sb("rgw", [128, LD, 2, 8, 128], BF16); wr = sb("wr", [128, LD, 8, NE], BF16)
        rb = sb("rb", [128, LD, NE])
        ring = sb("ring", [128, NSLOT, 6144], BF16)
        xT = sb("xT", [128, 8, T]); hT = sb("hT", [128, 8, T], BF16)
        xtok = sb("xtok", [128, 2, D]); scr = sb("scr", [128, 8192])
        BA = sb("BA", [128, 8, T], BF16); BB = sb("BB", [128, 8, T], BF16)
        rin = sb("rin", [128, 8, T + 3])
        mS = sb("mS", [128, T]); vS = sb("vS", [128, T]); rS = sb("rS", [128, T])
        st6 = sb("st6", [128, 2, 2, 6]); mv = sb("mv", [128, 2, 2]); rstd = sb("rstd", [128, 2])
        r_sc = sb("r_sc", [128, 2, NE]); r_bi = sb("r_bi", [128, 2, NE]); r_t = sb("r_t", [128, 2, NE])
        r_m1 = sb("r_m1", [128, 2, 8]); r_m2 = sb("r_m2", [128, 2, 8]); r_k = sb("r_k", [128, 2, 8])
        r_s8 = sb("r_s8", [128, 2, 8]); r_mb = sb("r_mb", [128, 2, NE]); r_e8 = sb("r_e8", [128, 2, 8])
        r_w = sb("r_w", [128, 2, NE]); r_d = sb("r_d", [128, 2]); r_G = sb("r_G", [128, 2, NE])
        GT = sb("GT", [128, T], BF16)
        tb = [sb("tb0", [128, 2, T]), sb("tb1", [128, 2, T])]
        hid = [sb("hid0", [128, 2, T], BF16), sb("hid1", [128, 2, T], BF16)]
        convh = sb("convh", [128, LD, 8, 3]); hst = sb("hst", [128, LD, 8])
        ps = es.enter_context(nc.psum_tensor("ps", [128, 4096], F32))

        sem_names = list(Prog.ENGS) + ["wl%d" % k for k in range(NSLOT)] + ["ws%d" % k for k in range(NSLOT)] + \
            ["sg%d" % k for k in range(4)] + ["pl", "xl", "st", "dbg"]
        sems = {s: es.enter_context(nc.semaphore(s)) for s in sem_names}

        Fv = [scr[:, i * 2048:(i + 1) * 2048].rearrange("p (m t) -> p m t", t=T) for i in range(4)]
        Fn = ["FA", "FB", "FC", "FD"]
        gB = scr[:].bitcast(BF16).rearrange("p (e t) -> p e t", t=T)
        vn = BB[:].rearrange("p m t -> p (m t)").rearrange("p (s c) -> p s c", s=2)
        STG = [scr[:, i * 2048:(i + 1) * 2048] for i in range(4)]

        def psh(i):
            return ps[:, i * 256:(i + 1) * 256]

        def psb(b):
            return ps[:, b * 512:(b + 1) * 512]

        def psn(lo, hi):
            return ["ps%d" % i for i in range(lo, hi)]

        def fres(k, banks=range(4)):
            return ["%s%d" % (Fn[k], b) for b in banks]

        def prmv(v, l):
            return prm[:, v // 4, (v % 4) * 32 + l * 8:(v % 4) * 32 + l * 8 + 8]

        def pv(name, l):
            return prmv(PV.index(name), l)

        def bc_t(ap8):
            return ap8.unsqueeze(2).to_broadcast([128, 8, T])

        P.op("pool", lambda e: e.memset(ident[:], 1.0), writes=["ident"])
        P.op("pool", lambda e: e.affine_select(out=ident[:], in_=ident[:], pattern=[[-1, 128]], compare_op=ALU.is_equal, fill=0.0, base=0, channel_multiplier=1), reads=["ident"], writes=["ident"])
        P.op("pool", lambda e: e.memset(tril[:], 1.0), writes=["tril"])
        P.op("pool", lambda e: e.affine_select(out=tril[:], in_=tril[:], pattern=[[-1, 128]], compare_op=ALU.is_ge, fill=0.0, base=0, channel_multiplier=1), reads=["tril"], writes=["tril"])
        P.op("pool", lambda e: e.memset(onesD[:], 1.0 / D), writes=["onesD"])
        P.op("pool", lambda e: e.memset(onesb[:], 1.0), writes=["onesb"])
        P.op("pool", lambda e: e.tensor_copy(out=identb[:], in_=ident[:]), reads=["ident"], writes=["identb"])
        P.op("pool", lambda e: e.memset(stgp[:], 0.0), writes=["stgp"])

        pl_ops = []
        ms = P.ops["pool"]
        stgp_init = ("pool", len(ms) - 1)

        def pload(v, l, src):
            b, o = v // 4, (v % 4) * 32 + l * 8
            pl_ops.append(P.op("sp", lambda e: e.dma_start(out=stgp[o:o + 8, b, :], in_=src), dma_sem="pl", after=[stgp_init]))
        for l in range(LD):
            for v, name in enumerate(PV):
                if name.startswith("cw"):
                    src = W["conv_w"][l, int(name[2]), :].rearrange("(j p) -> j p", p=128)
                elif name.startswith("ab"):
                    k = int(name[2])
                    src = W["ada_b"][l, k * D:(k + 1) * D].rearrange("(j p) -> j p", p=128)
                else:
                    src = W[name][l, :].rearrange("(j p) -> j p", p=128)
                pload(v, l, src)
        pl_ops.append(P.op("sp", lambda e: e.dma_start(out=stgp[0:nseq * 8, 5, :], in_=c_d.rearrange("s (j p) -> (s j) p", p=128)), dma_sem="pl", after=[stgp_init]))
        for b in range(6):
            P.op("pe", lambda e, b=b: e.transpose(out=psh(b)[:, 0:128], in_=stgp[:, b, :], identity=ident[:]), reads=["ident"], writes=["ps%d" % b], after=pl_ops)
            P.op("dve", lambda e, b=b: e.tensor_copy(out=prm[:, b, :], in_=psh(b)[:, 0:128]), reads=["ps%d" % b], writes=["prm"])
        ncol = nseq * 8
        P.op("act", lambda e: e.activation(out=cact[:, 0:ncol], in_=prm[:, 5, 0:ncol], func=AF.Tanh, scale=0.5), reads=["prm"], writes=["cact"])
        P.op("dve", lambda e: e.scalar_tensor_tensor(out=cact[:, 0:ncol], in0=cact[:, 0:ncol], scalar=1.0, in1=prm[:, 5, 0:ncol], op0=ALU.add, op1=ALU.mult), reads=["cact", "prm"], writes=["cact"])
        P.op("dve", lambda e: e.tensor_scalar(out=cact[:, 0:ncol], in0=cact[:, 0:ncol], scalar1=0.5, scalar2=None, op0=ALU.mult), reads=["cact"], writes=["cact"])
        for l in range(LD):
            P.op("act", lambda e, l=l: e.activation(out=spm[:, l, 0, :], in_=pv("rg_lambda", l), func=AF.Exp, scale=-1.0), reads=["prm"], writes=["spm"])
        for l in range(LD):
            P.op("act", lambda e, l=l: e.activation(out=spm[:, l, 0, :], in_=spm[:, l, 0, :], func=AF.Ln, bias=1.0), reads=["spm"], writes=["spm"])
        for l in range(LD):
            P.op("dve", lambda e, l=l: e.tensor_scalar(out=spm[:, l, 1, :], in0=spm[:, l, 0, :], scalar1=-8.0, scalar2=None, op0=ALU.mult), reads=["spm"], writes=["spm"])
            P.op("dve", lambda e, l=l: e.tensor_scalar(out=spm[:, l, 0, :], in0=spm[:, l, 0, :], scalar1=-4.0, scalar2=None, op0=ALU.mult), reads=["spm"], writes=["spm"])
            P.op("dve", lambda e, l=l: e.tensor_scalar(out=spm[:, l, 2, :], in0=pv("rg_ba", l), scalar1=0.5, scalar2=None, op0=ALU.mult), reads=["prm"], writes=["spm"])
            P.op("dve", lambda e, l=l: e.tensor_scalar(out=spm[:, l, 3, :], in0=pv("rg_bx", l), scalar1=0.5, scalar2=None, op0=ALU.mult), reads=["prm"], writes=["spm"])

        adaP = ps[:, 3072:3072 + LD * 48 * nseq].rearrange("p (l v m s) -> p l v m s", l=LD, v=6, m=8)
        adaPn = psn(12, 16)
        sgi = 0
        import os as _os
        _pha = _os.environ.get('PHA', '123')
        for l in (range(LD) if '2' in _pha else []):
            for q in range(24):
                k = sgi % 4
                sgi += 1
                stg = STG[k].rearrange("p (j n) -> p j n", n=256)
                src = W["ada_w"][l, :, q * 256:(q + 1) * 256].rearrange("(j p) n -> p j n", p=128)
                P.op("sp", lambda e, stg=stg, src=src: e.dma_start(out=stg, in_=src), writes=["stg%d" % k], dma_sem="sg%d" % k)
                for mm in range(2):
                    v, m = (q * 2 + mm) // 8, (q * 2 + mm) % 8
                    for j in range(8):
                        P.op("pe", lambda e, stg=stg, l=l, v=v, m=m, j=j, mm=mm: e.matmul(
                            adaP[:, l, v, m, :], lhsT=stg[:, j, mm * 128:(mm + 1) * 128],
                            rhs=(cact[:, j:j + 9:8] if nseq == 2 else cact[:, j:j + 1]), start=(j == 0), stop=(j == 7)),
                            reads=["stg%d" % k, "cact"], writes=adaPn)
        for l in (range(LD) if '2' in _pha else []):
            for k6 in range(6):
                vv = 14 + k6
                P.op("dve", lambda e, l=l, k6=k6, vv=vv: e.tensor_tensor(
                    out=adaS[:, l, k6, :, :], in0=adaP[:, l, k6, :, :],
                    in1=prmv(vv, l).unsqueeze(2).to_broadcast([128, 8, nseq]), op=ALU.add),
                    reads=adaPn + ["prm"], writes=["adaS"])
        for l in (range(LD) if '2' in _pha else []):
            for s in range(nseq):
                A = lambda k6, l=l, s=s: adaS[:, l, k6, :, s]
                o = lambda k, l=l, s=s: dv[:, l, s, k, :]
                P.op("dve", lambda e, A=A, o=o: e.tensor_scalar(out=o(0), in0=A(1), scalar1=1.0, scalar2=None, op0=ALU.add), reads=["adaS"], writes=["dv"])
                P.op("dve", lambda e, A=A, o=o: e.tensor_copy(out=o(1), in_=A(0)), reads=["adaS"], writes=["dv"])
                P.op("dve", lambda e, A=A, o=o: e.tensor_scalar(out=o(2), in0=A(2), scalar1=1.0 / (8.0 * ALPHA), scalar2=None, op0=ALU.mult), reads=["adaS"], writes=["dv"])
                P.op("dve", lambda e, A=A, o=o: e.tensor_scalar(out=o(5), in0=A(4), scalar1=1.0, scalar2=None, op0=ALU.add), reads=["adaS"], writes=["dv"])
                P.op("dve", lambda e, A=A, o=o, l=l: e.tensor_tensor(out=o(3), in0=o(5), in1=pv("ln1_g", l), op=ALU.mult), reads=["dv", "prm"], writes=["dv"])
                P.op("dve", lambda e, A=A, o=o, l=l: e.tensor_tensor(out=o(4), in0=o(5), in1=pv("ln1_b", l), op=ALU.mult), reads=["dv", "prm"], writes=["dv"])
                P.op("dve", lambda e, A=A, o=o: e.tensor_tensor(out=o(4), in0=o(4), in1=A(3), op=ALU.add), reads=["dv", "adaS"], writes=["dv"])
                P.op("dve", lambda e, A=A, o=o: e.tensor_scalar(out=o(5), in0=A(5), scalar1=1.0 / ALPHA, scalar2=None, op0=ALU.mult), reads=["adaS", "dv"], writes=["dv"])

        for l in (range(LD) if '3' in _pha else []):
            k = sgi % 4
            sgi += 1
            stg = STG[k][:, 0:1024].rearrange("p (g s) -> p g s", s=128)
            P.op("sp", lambda e, stg=stg, l=l: e.dma_start(out=stg, in_=W["sgu_w"][l].rearrange("g t s -> t g s")), writes=["stg%d" % k], dma_sem="sg%d" % k)
            P.op("pool", lambda e, stg=stg: e.tensor_tensor(out=stg, in0=stg, in1=tril[:].unsqueeze(1).to_broadcast([128, 8, 128]), op=ALU.mult), reads=["stg%d" % k, "tril"], writes=["stg%d" % k])
            for g in range(8):
                P.op("pe", lambda e, stg=stg, g=g: e.transpose(out=psh(g)[:, 0:128], in_=stg[:, g, :], identity=ident[:]), reads=["stg%d" % k, "ident"], writes=["ps%d" % g])
                P.op("act", lambda e, l=l, g=g: e.copy(out=WsT[:, l, g, :], in_=psh(g)[:, 0:128]), reads=["ps%d" % g], writes=["WsT"])
            k2 = sgi % 4
            sgi += 1
            stg2 = STG[k2][:, 0:1024]
            P.op("sp", lambda e, stg2=stg2, l=l: e.dma_start(out=stg2, in_=W["sgu_b"][l:l + 1].rearrange("o g t -> o (g t)").to_broadcast([128, 1024])), writes=["stg%d" % k2], dma_sem="sg%d" % k2)
            for g in range(8):
                P.op("pe", lambda e, l=l, g=g: e.matmul(psh(8 + g)[:, 0:128], lhsT=onesb[:], rhs=WsT[:, l, g, :], start=True, stop=True), reads=["onesb", "WsT"], writes=["ps%d" % (8 + g)])
                P.op("dve", lambda e, l=l, g=g, stg2=stg2: e.scalar_tensor_tensor(
                    out=bias2[:, l, g, :], in0=psh(8 + g)[:, 0:128], scalar=pv("sgu_ln_b", l)[:, g:g + 1],
                    in1=stg2[:, g * 128:(g + 1) * 128], op0=ALU.mult, op1=ALU.add),
                    reads=["ps%d" % (8 + g), "prm", "stg%d" % k2], writes=["bias2"])
            for wi, wn in enumerate(["rg_wa", "rg_wx"]):
                k3 = sgi % 4
                sgi += 1
                stg3 = STG[k3][:, 0:1024].rearrange("p (h j) -> p h j", j=128)
                P.op("sp", lambda e, stg3=stg3, l=l, wn=wn: e.dma_start(out=stg3, in_=W[wn][l].rearrange("h i j -> i h j")), writes=["stg%d" % k3], dma_sem="sg%d" % k3)
                P.op("dve", lambda e, stg3=stg3, l=l, wi=wi: e.tensor_copy(out=rgw[:, l, wi, :, :], in_=stg3), reads=["stg%d" % k3], writes=["rgw"])
            k4 = sgi % 4
            sgi += 1
            stg4 = STG[k4][:, 0:512].rearrange("p (j n) -> p j n", n=NE)
            P.op("sp", lambda e, stg4=stg4, l=l: e.dma_start(out=stg4, in_=W["router_w"][l].rearrange("(j p) n -> p j n", p=128)), writes=["stg%d" % k4], dma_sem="sg%d" % k4)
            P.op("dve", lambda e, stg4=stg4, l=l: e.tensor_copy(out=wr[:, l, :, :], in_=stg4), reads=["stg%d" % k4], writes=["wr"])
            P.op("sp", lambda e, l=l: e.dma_start(out=rb[:, l, :], in_=W["router_b"][l:l + 1, :].to_broadcast([128, NE])), writes=["rb"], dma_sem="pl")

        wstores = []
        first_wload = [True]
        MIXW = [("w_in", 1), ("w_in", 0), ("w_in", 3), ("w_in", 2), ("w_in", 4), ("w_in", 5),
                ("w_branch_a", None), ("w_branch_b", None), ("w_out", None)]
        cast_i = [0]
        slot_i = [0]

        sgi_box = [sgi]
        for l in (range(LD) if "B" in phases else []):
            for ci, (wn, slot_w) in enumerate(MIXW):
                for half in range(2):
                    slot = slot_i[0] % NSLOT
                    slot_i[0] += 1
                    for q in range(2):
                        c0 = half * 512 + q * 256
                        if wn == "w_in":
                            src = W[wn][l, :, slot_w * 1024 + c0: slot_w * 1024 + c0 + 256]
                        else:
                            src = W[wn][l, :, c0:c0 + 256]
                        src = src.rearrange("(j p) n -> p j n", p=128)
                        k = sgi_box[0] % 4
                        sgi_box[0] += 1
                        stgv = STG[k].rearrange("p (j n) -> p j n", n=256)
                        P.op("sp", lambda e, stgv=stgv, src=src: e.dma_start(out=stgv, in_=src), writes=["stg%d" % k], dma_sem="sg%d" % k)
                        dst = ring[:, slot, 0:4096].rearrange("p (j n) -> p j n", n=512)[:, :, q * 256:(q + 1) * 256]
                        eng = "act" if cast_i[0] % 2 == 0 else "dve"
                        cast_i[0] += 1
                        if eng == "act":
                            P.op("act", lambda e, dst=dst, stgv=stgv: e.copy(out=dst, in_=stgv), reads=["stg%d" % k], writes=["ring%d" % slot])
                        else:
                            P.op("dve", lambda e, dst=dst, stgv=stgv: e.tensor_copy(out=dst, in_=stgv), reads=["stg%d" % k], writes=["ring%d" % slot])
                    wstores.append(P.op("sp", lambda e, slot=slot, l=l, ci=ci, half=half: e.dma_start(out=wmix_d[l][ci * 2 + half], in_=ring[:, slot, 0:4096]),
                         reads=["ring%d" % slot], dma_sem="ws%d" % slot))
            for ex in range(NE + 1):
                slot = slot_i[0] % NSLOT
                slot_i[0] += 1
                if ex < NE:
                    s1, s3, s2 = W["exp_w1"][l, ex], W["exp_w3"][l, ex], W["exp_w2"][l, ex]
                else:
                    s1, s3, s2 = W["sh_w1"][l], W["sh_w3"][l], W["sh_w2"][l]
                for q, src in enumerate([s1, s3]):
                    src = src.rearrange("(j p) n -> p j n", p=128)
                    k = sgi_box[0] % 4
                    sgi_box[0] += 1
                    stgv = STG[k].rearrange("p (j n) -> p j n", n=256)
                    P.op("sp", lambda e, stgv=stgv, src=src: e.dma_start(out=stgv, in_=src), writes=["stg%d" % k], dma_sem="sg%d" % k)
                    dst = ring[:, slot, 0:4096].rearrange("p (j n) -> p j n", n=512)[:, :, q * 256:(q + 1) * 256]
                    eng = "act" if cast_i[0] % 2 == 0 else "dve"
                    cast_i[0] += 1
                    if eng == "act":
                        P.op("act", lambda e, dst=dst, stgv=stgv: e.copy(out=dst, in_=stgv), reads=["stg%d" % k], writes=["ring%d" % slot])
                    else:
                        P.op("dve", lambda e, dst=dst, stgv=stgv: e.tensor_copy(out=dst, in_=stgv), reads=["stg%d" % k], writes=["ring%d" % slot])
                src = s2.rearrange("(f p) o -> p f o", p=128)
                k = sgi_box[0] % 4
                sgi_box[0] += 1
                stgv = STG[k].rearrange("p (f o) -> p f o", f=2)
                P.op("sp", lambda e, stgv=stgv, src=src: e.dma_start(out=stgv, in_=src), writes=["stg%d" % k], dma_sem="sg%d" % k)
                dst = ring[:, slot, 4096:6144].rearrange("p (f o) -> p f o", f=2)
                eng = "act" if cast_i[0] % 2 == 0 else "dve"
                cast_i[0] += 1
                if eng == "act":
                    P.op("act", lambda e, dst=dst, stgv=stgv: e.copy(out=dst, in_=stgv), reads=["stg%d" % k], writes=["ring%d" % slot])
                else:
                    P.op("dve", lambda e, dst=dst, stgv=stgv: e.tensor_copy(out=dst, in_=stgv), reads=["stg%d" % k], writes=["ring%d" % slot])
                wstores.append(P.op("sp", lambda e, slot=slot, l=l, ex=ex: e.dma_start(out=wexp_d[l][ex], in_=ring[:, slot, :]),
                     reads=["ring%d" % slot], dma_sem="ws%d" % slot))

        def wload(src, width):
            slot = slot_i[0] % NSLOT
            slot_i[0] += 1
            aft = wstores if first_wload[0] else ()
            first_wload[0] = False
            P.op("sp", lambda e: e.dma_start(out=ring[:, slot, 0:width], in_=src),
                 writes=["ring%d" % slot], dma_sem="wl%d" % slot, after=aft)
            return slot

        def gelu2(src, dst, rsrc, rdst):
            P.op("act", lambda e: e.activation(out=dst, in_=src, func=AF.Square, scale=SQ_G), reads=rsrc, writes=rdst)
            P.op("dve", lambda e: e.scalar_tensor_tensor(out=dst, in0=dst, scalar=1.0, in1=src, op0=ALU.add, op1=ALU.mult), reads=rsrc + rdst, writes=rdst)
            P.op("act", lambda e: e.activation(out=dst, in_=dst, func=AF.Tanh, scale=C_G), reads=rdst, writes=rdst)
            P.op("dve", lambda e: e.scalar_tensor_tensor(out=dst, in0=dst, scalar=1.0, in1=src, op0=ALU.add, op1=ALU.mult), reads=rsrc + rdst, writes=rdst)

        def fm_matmul(l, ci, rhs_ap, rhs_res, ps_base):
            for half in range(2):
                slot = wload(wmix_d[l][ci * 2 + half], 4096)
                wv = ring[:, slot, 0:4096].rearrange("p (j n) -> p j n", n=512)
                for mm in range(4):
                    m = half * 4 + mm
                    for j in range(8):
                        P.op("pe", lambda e, wv=wv, mm=mm, m=m, j=j: e.matmul(
                            psh(ps_base + m), lhsT=wv[:, j, mm * 128:(mm + 1) * 128], rhs=rhs_ap[:, j, :],
                            start=(j == 0), stop=(j == 7)),
                            reads=["ring%d" % slot] + rhs_res, writes=["ps%d" % (ps_base + m)])

        def layer_norm_fm(res, res_names, sq, sq_names, eps):
            for b in range(4):
                P.op("act", lambda e, b=b: e.activation(out=sq[:, 2 * b:2 * b + 2, :], in_=res[:, 2 * b:2 * b + 2, :], func=AF.Square),
                     reads=[res_names[b]], writes=[sq_names[b]])
            for j in range(8):
                P.op("pe", lambda e, j=j: e.matmul(psh(0), lhsT=onesD[:], rhs=res[:, j, :], start=(j == 0), stop=(j == 7)),
                     reads=["onesD", res_names[j // 2]], writes=["ps0"])
            for j in range(8):
                P.op("pe", lambda e, j=j: e.matmul(psh(1), lhsT=onesD[:], rhs=sq[:, j, :], start=(j == 0), stop=(j == 7)),
                     reads=["onesD", sq_names[j // 2]], writes=["ps1"])
            P.op("act", lambda e: e.copy(out=mS[:], in_=psh(0)), reads=["ps0"], writes=["mS"])
            P.op("dve", lambda e: e.tensor_tensor(out=vS[:], in0=mS[:], in1=psh(0), op=ALU.mult), reads=["mS", "ps0"], writes=["vS"])
            P.op("dve", lambda e: e.scalar_tensor_tensor(out=vS[:], in0=vS[:], scalar=-1.0, in1=psh(1), op0=ALU.mult, op1=ALU.add), reads=["vS", "ps1"], writes=["vS"])
            P.op("act", lambda e: e.activation(out=rS[:], in_=vS[:], func=AF.Sqrt, bias=eps), reads=["vS"], writes=["rS"])
            P.op("dve", lambda e: e.reciprocal(out=rS[:], in_=rS[:]), reads=["rS"], writes=["rS"])
            for b in range(4):
                P.op("pool", lambda e, b=b: e.tensor_tensor(out=res[:, 2 * b:2 * b + 2, :], in0=res[:, 2 * b:2 * b + 2, :],
                     in1=mS[:].unsqueeze(1).to_broadcast([128, 2, T]), op=ALU.subtract), reads=[res_names[b], "mS"], writes=[res_names[b]])
                P.op("pool", lambda e, b=b: e.tensor_tensor(out=res[:, 2 * b:2 * b + 2, :], in0=res[:, 2 * b:2 * b + 2, :],
                     in1=rS[:].unsqueeze(1).to_broadcast([128, 2, T]), op=ALU.mult), reads=[res_names[b], "rS"], writes=[res_names[b]])

        def affine_fm(src, src_names, tmp, tmp_names, dst, dst_name, g8, b8, pres):
            P.op("dve", lambda e: e.tensor_tensor(out=tmp, in0=src, in1=bc_t(g8), op=ALU.mult), reads=src_names + pres, writes=tmp_names)
            P.op("pool", lambda e: e.tensor_tensor(out=dst, in0=tmp, in1=bc_t(b8), op=ALU.add), reads=tmp_names + pres, writes=[dst_name])

        def dump(name, ap, res):
            if name in dbg_d:
                P.op("sp", lambda e: e.dma_start(out=dbg_d[name], in_=ap), reads=res, dma_sem="dbg")

        FA, FB, FC, FD = Fv
        FA4 = scr[:, 0:2048].rearrange("p (m s t) -> p m s t", m=8, s=2)

        for s in (range(nseq) if "C" in phases else []):
            P.op("pool", lambda e: e.memset(convh[:], 0.0), reads=["convh"], writes=["convh"])
            P.op("pool", lambda e: e.memset(hst[:], 0.0), reads=["hst"], writes=["hst"])
            for it in range(nt):
                t0 = it * T
                P.op("sp", lambda e, s=s, t0=t0: e.dma_start(out=xtok[:], in_=x_d[s, t0:t0 + T, :].rearrange("(u p) d -> p u d", p=128)),
                     writes=["vg0", "vg1", "vg2", "vg3"], dma_sem="xl")
                for m in range(8):
                    for u in range(2):
                        P.op("pe", lambda e, m=m, u=u: e.transpose(out=psh(m)[:, u * 128:(u + 1) * 128], in_=xtok[:, u, m * 128:(m + 1) * 128], identity=ident[:]),
                             reads=["vg%d" % (u * 2 + m // 4), "ident"], writes=["ps%d" % m])
                for b in range(4):
                    P.op("act", lambda e, b=b: e.copy(out=xT[:, 2 * b:2 * b + 2, :], in_=psb(b).rearrange("p (m t) -> p m t", t=T)),
                         reads=psn(2 * b, 2 * b + 2), writes=["xT"])
                for l in range(LD):
                    d = lambda k, l=l, s=s: dv[:, l, s, k, :]
                    affine_fm(xT[:], ["xT"], FD, fres(3), hT[:], "hT", d(0), d(1), ["dv"])
                    for half in range(2):
                        slot = wload(wmix_d[l][0 * 2 + half], 4096)
                        wv = ring[:, slot, 0:4096].rearrange("p (j n) -> p j n", n=512)
                        for u in range(2):
                            for j in range(8):
                                P.op("pe", lambda e, wv=wv, u=u, j=j, half=half: e.matmul(
                                    psb(u * 2 + half), lhsT=hT[:, j, u * 128:(u + 1) * 128], rhs=wv[:, j, :], start=(j == 0), stop=(j == 7)),
                                    reads=["ring%d" % slot, "hT"], writes=psn(2 * (u * 2 + half), 2 * (u * 2 + half) + 2))
                    for u in range(2):
                        for half in range(2):
                            b = u * 2 + half
                            gelu2(psb(b), xtok[:, u, half * 512:(half + 1) * 512], psn(2 * b, 2 * b + 2), ["vg%d" % b])
                            P.op("dve", lambda e, u=u, half=half: e.bn_stats(out=st6[:, u, half, :], in_=xtok[:, u, half * 512:(half + 1) * 512]),
                                 reads=["vg%d" % b], writes=["st6_%d" % b])
                        P.op("dve", lambda e, u=u: e.bn_aggr(out=mv[:, u, :], in_=st6[:, u, :, :].rearrange("p a b -> p (a b)")),
                             reads=["st6_%d" % (u * 2), "st6_%d" % (u * 2 + 1)], writes=["mv%d" % u])
                    P.op("act", lambda e: e.activation(out=rstd[:], in_=mv[:, :, 1], func=AF.Sqrt, bias=4.0 * EPS), reads=["mv0", "mv1"], writes=["rstd"])
                    P.op("dve", lambda e: e.reciprocal(out=rstd[:], in_=rstd[:]), reads=["rstd"], writes=["rstd"])
                    for u in range(2):
                        P.op("dve", lambda e, u=u: e.tensor_scalar(out=vn[:, u, :], in0=xtok[:, u, :], scalar1=mv[:, u, 0:1], scalar2=rstd[:, u:u + 1],
                             op0=ALU.subtract, op1=ALU.mult), reads=["vg%d" % (2 * u), "vg%d" % (2 * u + 1), "mv%d" % u, "rstd"], writes=["BB%d" % (2 * u), "BB%d" % (2 * u + 1)])
                    for g in range(8):
                        for u in range(2):
                            P.op("pe", lambda e, g=g, u=u, l=l: e.matmul(psh(8 + g)[:, u * 128:(u + 1) * 128], lhsT=vn[:, u, g * 128:(g + 1) * 128],
                                 rhs=WsT[:, l, g, :], start=True, stop=True), reads=["BB%d" % (2 * u + g // 4), "WsT"], writes=["ps%d" % (8 + g)])
                        P.op("dve", lambda e, g=g, l=l: e.scalar_tensor_tensor(
                            out=FA4[:, g, :, :], in0=psh(8 + g).rearrange("p (s t) -> p s t", s=2), scalar=pv("sgu_ln_g", l)[:, g:g + 1],
                            in1=bias2[:, l, g, :].unsqueeze(1).to_broadcast([128, 2, 128]), op0=ALU.mult, op1=ALU.add),
                            reads=["ps%d" % (8 + g), "prm", "bias2"], writes=["FA%d" % (g // 2)])
                    fm_matmul(l, 1, hT, ["hT"], 0)
                    for b in range(4):
                        gelu2(psb(b), FB[:, 2 * b:2 * b + 2, :].rearrange("p m t -> p (m t)"), psn(2 * b, 2 * b + 2), ["FB%d" % b])
                        P.op("pool", lambda e, b=b: e.tensor_tensor(out=BA[:, 2 * b:2 * b + 2, :], in0=FB[:, 2 * b:2 * b + 2, :], in1=FA[:, 2 * b:2 * b + 2, :], op=ALU.mult),
                             reads=["FB%d" % b, "FA%d" % b], writes=["BA%d" % b])
                    fm_matmul(l, 2, hT, ["hT"], 8)
                    P.op("pool", lambda e, l=l: e.tensor_copy(out=rin[:, :, 0:3], in_=convh[:, l, :, :]), reads=["convh"], writes=["rinh"])
                    for b in range(4):
                        P.op("act", lambda e, b=b: e.copy(out=rin[:, 2 * b:2 * b + 2, 3:3 + T], in_=psb(4 + b).rearrange("p (m t) -> p m t", t=T)),
                             reads=psn(8 + 2 * b, 10 + 2 * b), writes=["rin%d" % b])
                    rin_all = ["rinh"] + ["rin%d" % b for b in range(4)]
                    P.op("pool", lambda e, l=l: e.tensor_copy(out=convh[:, l, :, :], in_=rin[:, :, T:T + 3]), reads=rin_all + ["convh"], writes=["convh"])
                    P.op("pool", lambda e, l=l: e.tensor_tensor(out=FB, in0=rin[:, :, 0:T], in1=bc_t(pv("cw0", l)), op=ALU.mult), reads=rin_all + ["prm"], writes=fres(1))
                    P.op("pool", lambda e, l=l: e.tensor_tensor(out=FB, in0=FB, in1=bc_t(pv("conv_b", l)), op=ALU.add), reads=fres(1) + ["prm"], writes=fres(1))
                    for k in range(1, 4):
                        P.op("pool", lambda e, l=l, k=k: e.tensor_tensor(out=FC, in0=rin[:, :, k:k + T], in1=bc_t(pv("cw%d" % k, l)), op=ALU.mult), reads=rin_all + ["prm"], writes=fres(2))
                        P.op("pool", lambda e: e.tensor_tensor(out=FB, in0=FB, in1=FC, op=ALU.add), reads=fres(1) + fres(2), writes=fres(1))
                    P.op("act", lambda e: e.copy(out=BB[:], in_=FB), reads=fres(1), writes=["BB%d" % b for b in range(4)])
                    for wi in range(2):
                        for h in range(8):
                            P.op("pe", lambda e, l=l, wi=wi, h=h: e.matmul(psh(wi * 8 + h), lhsT=rgw[:, l, wi, h, :], rhs=BB[:, h, :], start=True, stop=True),
                                 reads=["rgw", "BB%d" % (h // 2)], writes=["ps%d" % (wi * 8 + h)])
                    for h in range(8):
                        P.op("act", lambda e, l=l, h=h: e.activation(out=FC[:, h, :], in_=psh(h), func=AF.Tanh, scale=0.5, bias=spm[:, l, 2, h:h + 1]),
                             reads=["ps%d" % h, "spm"], writes=["FC%d" % (h // 2)])
                        P.op("act", lambda e, l=l, h=h: e.activation(out=FD[:, h, :], in_=FC[:, h, :], func=AF.Exp, scale=spm[:, l, 0, h:h + 1], bias=spm[:, l, 0, h:h + 1]),
                             reads=["FC%d" % (h // 2), "spm"], writes=["FD%d" % (h // 2)])
                        P.op("act", lambda e, l=l, h=h: e.activation(out=FC[:, h, :], in_=FC[:, h, :], func=AF.Exp, scale=spm[:, l, 1, h:h + 1], bias=spm[:, l, 1, h:h + 1]),
                             reads=["FC%d" % (h // 2), "spm"], writes=["FC%d" % (h // 2)])
                        P.op("act", lambda e, l=l, h=h: e.activation(out=FA[:, h, :], in_=psh(8 + h), func=AF.Tanh, scale=0.5, bias=spm[:, l, 3, h:h + 1]),
                             reads=["ps%d" % (8 + h), "spm"], writes=["FA%d" % (h // 2)])
                    P.op("pool", lambda e: e.tensor_scalar(out=FC, in0=FC, scalar1=-1.0, scalar2=1.0, op0=ALU.mult, op1=ALU.add), reads=fres(2), writes=fres(2))
                    P.op("act", lambda e: e.activation(out=FC, in_=FC, func=AF.Sqrt), reads=fres(2), writes=fres(2))
                    P.op("dve", lambda e: e.scalar_tensor_tensor(out=FA, in0=FA, scalar=1.0, in1=FB, op0=ALU.add, op1=ALU.mult), reads=fres(0) + fres(1), writes=fres(0))
                    P.op("pool", lambda e: e.tensor_tensor(out=FC, in0=FC, in1=FA, op=ALU.mult), reads=fres(2) + fres(0), writes=fres(2))
                    for h in range(8):
                        P.op("dve", lambda e, l=l, h=h: e.tensor_tensor_scan(out=FA[:, h, :], data0=FD[:, h, :], data1=FC[:, h, :], initial=hst[:, l, h:h + 1], op0=ALU.mult, op1=ALU.add),
                             reads=["FD%d" % (h // 2), "FC%d" % (h // 2), "hst", "FA%d" % (h // 2)], writes=["FA%d" % (h // 2)])
                    P.op("pool", lambda e, l=l: e.tensor_copy(out=hst[:, l, :], in_=FA[:, :, T - 1]), reads=fres(0) + ["hst"], writes=["hst"])
                    fm_matmul(l, 3, hT, ["hT"], 0)
                    for b in range(4):
                        gelu2(psb(b), FB[:, 2 * b:2 * b + 2, :].rearrange("p m t -> p (m t)"), psn(2 * b, 2 * b + 2), ["FB%d" % b])
                        P.op("pool", lambda e, b=b: e.tensor_tensor(out=BB[:, 2 * b:2 * b + 2, :], in0=FB[:, 2 * b:2 * b + 2, :], in1=FA[:, 2 * b:2 * b + 2, :], op=ALU.mult),
                             reads=["FB%d" % b, "FA%d" % b], writes=["BB%d" % b])
                    fm_matmul(l, 4, hT, ["hT"], 8)
                    for b in range(4):
                        P.op("act", lambda e, b=b: e.activation(out=FC[:, 2 * b:2 * b + 2, :], in_=psb(4 + b).rearrange("p (m t) -> p m t", t=T), func=AF.Tanh, scale=0.5),
                             reads=psn(8 + 2 * b, 10 + 2 * b), writes=["FC%d" % b])
                    fm_matmul(l, 5, hT, ["hT"], 0)
                    for b in range(4):
                        P.op("act", lambda e, b=b: e.activation(out=FD[:, 2 * b:2 * b + 2, :], in_=psb(b).rearrange("p (m t) -> p m t", t=T), func=AF.Tanh, scale=0.5),
                             reads=psn(2 * b, 2 * b + 2), writes=["FD%d" % b])
                    fm_matmul(l, 6, BA, ["BA%d" % b for b in range(4)], 8)
                    for b in range(4):
                        P.op("dve", lambda e, b=b: e.scalar_tensor_tensor(out=FC[:, 2 * b:2 * b + 2, :], in0=FC[:, 2 * b:2 * b + 2, :], scalar=1.0,
                             in1=psb(4 + b).rearrange("p (m t) -> p m t", t=T), op0=ALU.add, op1=ALU.mult), reads=["FC%d" % b] + psn(8 + 2 * b, 10 + 2 * b), writes=["FC%d" % b])
                    fm_matmul(l, 7, BB, ["BB%d" % b for b in range(4)], 0)
                    for b in range(4):
                        P.op("dve", lambda e, b=b: e.scalar_tensor_tensor(out=FD[:, 2 * b:2 * b + 2, :], in0=FD[:, 2 * b:2 * b + 2, :], scalar=1.0,
                             in1=psb(b).rearrange("p (m t) -> p m t", t=T), op0=ALU.add, op1=ALU.mult), reads=["FD%d" % b] + psn(2 * b, 2 * b + 2), writes=["FD%d" % b])
                        P.op("dve", lambda e, b=b: e.scalar_tensor_tensor(out=BA[:, 2 * b:2 * b + 2, :], in0=FC[:, 2 * b:2 * b + 2, :], scalar=2.0,
                             in1=FD[:, 2 * b:2 * b + 2, :], op0=ALU.mult, op1=ALU.add), reads=["FC%d" % b, "FD%d" % b], writes=["BA%d" % b])
                    fm_matmul(l, 8, BA, ["BA%d" % b for b in range(4)], 8)
                    for m in range(8):
                        P.op("dve", lambda e, m=m, d=d: e.scalar_tensor_tensor(out=FC[:, m, :], in0=psh(8 + m), scalar=d(2)[:, m:m + 1], in1=xT[:, m, :], op0=ALU.mult, op1=ALU.add),
                             reads=["ps%d" % (8 + m), "dv", "xT"], writes=["FC%d" % (m // 2)])
                    layer_norm_fm(FC, fres(2), FD, fres(3), EPS / (ALPHA * ALPHA))
                    affine_fm(FC, fres(2), FD, fres(3), xT[:], "xT", pv("ln1_g", l), pv("ln1_b", l), ["prm"])
                    affine_fm(FC, fres(2), FA, fres(0), hT[:], "hT", d(3), d(4), ["dv"])
                    if l == 0 and it == 0 and s == 0:
                        dump("x1", xT[:], ["xT"])
                    for u in range(2):
                        for j in range(8):
                            P.op("pe", lambda e, u=u, j=j, l=l: e.matmul(psh(2)[:, u * NE:(u + 1) * NE], lhsT=hT[:, j, u * 128:(u + 1) * 128], rhs=wr[:, l, j, :], start=(j == 0), stop=(j == 7)),
                                 reads=["hT", "wr"], writes=["ps2"])
                    lg = psh(2)[:, 0:2 * NE].rearrange("p (u n) -> p u n", u=2)
                    P.op("act", lambda e: e.activation(out=r_sc[:], in_=lg, func=AF.Tanh, scale=0.5), reads=["ps2"], writes=["r_sc"])
                    P.op("dve", lambda e: e.tensor_scalar(out=r_sc[:], in0=r_sc[:], scalar1=0.5, scalar2=0.5, op0=ALU.mult, op1=ALU.add), reads=["r_sc"], writes=["r_sc"])
                    P.op("dve", lambda e, l=l: e.tensor_tensor(out=r_bi[:], in0=r_sc[:], in1=rb[:, l, :].unsqueeze(1).to_broadcast([128, 2, NE]), op=ALU.add), reads=["r_sc", "rb"], writes=["r_bi"])
                    bi4 = r_bi[:].rearrange("p u (g k) -> p u g k", k=8)
                    P.op("dve", lambda e: e.tensor_reduce(out=r_m1[:], in_=bi4, axis=AX.X, op=ALU.max), reads=["r_bi"], writes=["r_m1"])
                    t4 = r_t[:].rearrange("p u (g k) -> p u g k", k=8)
                    P.op("dve", lambda e: e.tensor_tensor(out=t4, in0=bi4, in1=r_m1[:].unsqueeze(3).to_broadcast([128, 2, 8, 8]), op=ALU.is_ge), reads=["r_bi", "r_m1"], writes=["r_t"])
                    P.op("dve", lambda e: e.scalar_tensor_tensor(out=r_t[:], in0=r_t[:], scalar=-1e9, in1=r_bi[:], op0=ALU.mult, op1=ALU.add), reads=["r_t", "r_bi"], writes=["r_t"])
                    P.op("dve", lambda e: e.tensor_reduce(out=r_m2[:], in_=t4, axis=AX.X, op=ALU.max), reads=["r_t"], writes=["r_m2"])
                    P.op("dve", lambda e: e.tensor_tensor(out=r_m1[:], in0=r_m1[:], in1=r_m2[:], op=ALU.add), reads=["r_m1", "r_m2"], writes=["r_m1"])
                    for u in range(2):
                        P.op("dve", lambda e, u=u: e.max(out=r_s8[:, u, :], in_=r_m1[:, u, :]), reads=["r_m1"], writes=["r_s8"])
                    for u in range(2):
                        P.op("dve", lambda e, u=u: e.tensor_scalar(out=r_k[:, u, :], in0=r_m1[:, u, :], scalar1=r_s8[:, u, 3:4], scalar2=None, op0=ALU.is_ge), reads=["r_m1", "r_s8"], writes=["r_k"])
                    kb = r_k[:].unsqueeze(3).to_broadcast([128, 2, 8, 8])
                    mb4 = r_mb[:].rearrange("p u (g k) -> p u g k", k=8)
                    P.op("dve", lambda e: e.tensor_tensor(out=mb4, in0=bi4, in1=kb, op=ALU.mult), reads=["r_bi", "r_k"], writes=["r_mb"])
                    P.op("dve", lambda e: e.tensor_scalar(out=r_k[:], in0=r_k[:], scalar1=1e9, scalar2=-1e9, op0=ALU.mult, op1=ALU.add), reads=["r_k", "r_mb"], writes=["r_k"])
                    P.op("dve", lambda e: e.tensor_tensor(out=mb4, in0=mb4, in1=kb, op=ALU.add), reads=["r_mb", "r_k"], writes=["r_mb"])
                    for u in range(2):
                        P.op("dve", lambda e, u=u: e.max(out=r_e8[:, u, :], in_=r_mb[:, u, :]), reads=["r_mb"], writes=["r_e8"])
                    for u in range(2):
                        P.op("dve", lambda e, u=u: e.tensor_scalar(out=r_w[:, u, :], in0=r_mb[:, u, :], scalar1=r_e8[:, u, 7:8], scalar2=None, op0=ALU.is_ge), reads=["r_mb", "r_e8"], writes=["r_w"])
                    P.op("dve", lambda e: e.tensor_tensor(out=r_w[:], in0=r_w[:], in1=r_sc[:], op=ALU.mult), reads=["r_w", "r_sc"], writes=["r_w"])
                    P.op("dve", lambda e: e.tensor_reduce(out=r_d[:], in_=r_w[:], axis=AX.X, op=ALU.add), reads=["r_w"], writes=["r_d"])
                    P.op("dve", lambda e: e.reciprocal(out=r_d[:], in_=r_d[:]), reads=["r_d"], writes=["r_d"])
                    for u in range(2):
                        P.op("dve", lambda e, u=u: e.tensor_scalar(out=r_G[:, u, :], in0=r_w[:, u, :], scalar1=r_d[:, u:u + 1], scalar2=1.25, op0=ALU.mult, op1=ALU.mult), reads=["r_w", "r_d"], writes=["r_G"])
                    for u in range(2):
                        P.op("pe", lambda e, u=u: e.transpose(out=psh(3)[0:NE, u * 128:(u + 1) * 128], in_=r_G[:, u, :], identity=ident[:]), reads=["r_G", "ident"], writes=["ps3"])
                    P.op("act", lambda e: e.copy(out=GT[0:NE, :], in_=psh(3)[0:NE, :]), reads=["ps3"], writes=["GT"])
                    for ep in range(NE // 2):
                        hb = 4 + (ep % 2)
                        bank = 2 + (ep % 2)
                        for q in range(2):
                            ex = ep * 2 + q
                            P.op("pe", lambda e, ex=ex, bank=bank, q=q: e.matmul(psb(bank)[:, q * T:(q + 1) * T], lhsT=identb[0:NE, ex:ex + 1].to_broadcast([NE, 128]), rhs=GT[0:NE, :], start=True, stop=True),
                                 reads=["identb", "GT"], writes=["ps%d" % (2 * bank + q)])
                        eng = "act" if ep % 2 == 0 else "dve"
                        fk = ep // 8
                        if eng == "act":
                            P.op("act", lambda e, ep=ep, bank=bank: e.copy(out=gB[:, 2 * ep:2 * ep + 2, :], in_=psb(bank).rearrange("p (q t) -> p q t", t=T)),
                                 reads=psn(2 * bank, 2 * bank + 2), writes=["gB%d" % ep] + fres(fk))
                        else:
                            P.op("dve", lambda e, ep=ep, bank=bank: e.tensor_copy(out=gB[:, 2 * ep:2 * ep + 2, :], in_=psb(bank).rearrange("p (q t) -> p q t", t=T)),
                                 reads=psn(2 * bank, 2 * bank + 2), writes=["gB%d" % ep] + fres(fk))
                    slots = {}

                    def up(ex):
                        p = ex % 2
                        slot = wload(wexp_d[l][ex], 6144)
                        slots[ex] = slot
                        w13 = ring[:, slot, 0:4096].rearrange("p (j n) -> p j n", n=512)
                        for which in range(2):
                            for fc in range(2):
                                for j in range(8):
                                    P.op("pe", lambda e, w13=w13, which=which, fc=fc, j=j, p=p: e.matmul(
                                        psb(2 * p + which)[:, fc * T:(fc + 1) * T], lhsT=w13[:, j, which * 256 + fc * 128: which * 256 + (fc + 1) * 128],
                                        rhs=hT[:, j, :], start=(j == 0), stop=(j == 7)),
                                        reads=["ring%d" % slot, "hT"], writes=["ps%d" % (2 * (2 * p + which) + fc)])
                        h1 = psb(2 * p).rearrange("p (f t) -> p f t", t=T)
                        h3 = psb(2 * p + 1).rearrange("p (f t) -> p f t", t=T)
                        h1n, h3n = psn(4 * p, 4 * p + 2), psn(4 * p + 2, 4 * p + 4)
                        P.op("act", lambda e: e.activation(out=tb[p][:], in_=h1, func=AF.Tanh, scale=0.5), reads=h1n, writes=["tb%d" % p])
                        P.op("dve", lambda e: e.scalar_tensor_tensor(out=tb[p][:], in0=tb[p][:], scalar=1.0, in1=h1, op0=ALU.add, op1=ALU.mult), reads=["tb%d" % p] + h1n, writes=["tb%d" % p])
                        if ex < NE:
                            P.op("dve", lambda e: e.tensor_tensor(out=tb[p][:], in0=tb[p][:], in1=h3, op=ALU.mult), reads=["tb%d" % p] + h3n, writes=["tb%d" % p])
                            P.op("pool", lambda e: e.tensor_tensor(out=hid[p][:], in0=tb[p][:], in1=gB[:, ex, :].unsqueeze(1).to_broadcast([128, 2, T]), op=ALU.mult),
                                 reads=["tb%d" % p, "gB%d" % (ex // 2)] + fres(ex // 16), writes=["hid%d" % p])
                        else:
                            P.op("dve", lambda e: e.scalar_tensor_tensor(out=hid[p][:], in0=tb[p][:], scalar=0.5, in1=h3, op0=ALU.mult, op1=ALU.mult), reads=["tb%d" % p] + h3n, writes=["hid%d" % p])

                    def down(ex):
                        p = ex % 2
                        slot = slots[ex]
                        w2 = ring[:, slot, 4096:6144].rearrange("p (f o) -> p f o", f=2)
                        for m in range(8):
                            for fc in range(2):
                                P.op("pe", lambda e, w2=w2, m=m, fc=fc, p=p: e.matmul(psh(8 + m), lhsT=w2[:, fc, m * 128:(m + 1) * 128], rhs=hid[p][:, fc, :],
                                     start=(ex == 0 and fc == 0 and m % 2 == 0), stop=(ex == NE and fc == 1), skip_group_check=True),
                                     reads=["ring%d" % slot, "hid%d" % p], writes=["ps%d" % (8 + m)])
                    up(0)
                    for ex in range(1, NE + 1):
                        up(ex)
                        down(ex - 1)
                    down(NE)
                    for m in range(8):
                        P.op("dve", lambda e, m=m, d=d: e.scalar_tensor_tensor(out=FC[:, m, :], in0=psh(8 + m), scalar=d(5)[:, m:m + 1], in1=xT[:, m, :], op0=ALU.mult, op1=ALU.add),
                             reads=["ps%d" % (8 + m), "dv", "xT"], writes=["FC%d" % (m // 2)])
                    layer_norm_fm(FC, fres(2), FD, fres(3), EPS / (ALPHA * ALPHA))
                    affine_fm(FC, fres(2), FD, fres(3), xT[:], "xT", pv("ln2_g", l), pv("ln2_b", l), ["prm"])
                    if l == 0 and it == 0 and s == 0:
                        dump("x2", xT[:], ["xT"])
                for u in range(2):
                    for m in range(8):
                        P.op("pe", lambda e, u=u, m=m: e.transpose(out=psb(u * 2 + m // 4)[:, (m % 4) * 128:(m % 4 + 1) * 128], in_=xT[:, m, u * 128:(u + 1) * 128], identity=ident[:]),
                             reads=["xT", "ident"], writes=psn(2 * (u * 2 + m // 4), 2 * (u * 2 + m // 4) + 2))
                    for half in range(2):
                        b = u * 2 + half
                        if half == 0:
                            P.op("act", lambda e, u=u, half=half, b=b: e.copy(out=xtok[:, u, half * 512:(half + 1) * 512], in_=psb(b)), reads=psn(2 * b, 2 * b + 2) + ["vg%d" % b], writes=["vg%d" % b])
                        else:
                            P.op("dve", lambda e, u=u, half=half, b=b: e.tensor_copy(out=xtok[:, u, half * 512:(half + 1) * 512], in_=psb(b)), reads=psn(2 * b, 2 * b + 2) + ["vg%d" % b], writes=["vg%d" % b])
                P.op("sp", lambda e, s=s, t0=t0: e.dma_start(out=out_d[s, t0:t0 + T, :].rearrange("(u p) d -> p u d", p=128), in_=xtok[:]),
                     reads=["vg%d" % b for b in range(4)], dma_sem="st")

        P.finalize()
        with nc.Block() as block:
            @block.sync
            def _(e):
                P.emit("sp", e, sems)
                for s_, v_ in P.dma_sem_count.items():
                    if s_ in ("st", "dbg"):
                        e.wait_ge(sems[s_], v_)

            @block.scalar
            def _(e):
                P.emit("act", e, sems)

            @block.vector
            def _(e):
                P.emit("dve", e, sems)

            @block.gpsimd
            def _(e):
                P.emit("pool", e, sems)

            @block.tensor
            def _(e):
                P.emit("pe", e, sems)
    return nc, P


def kernel(**inputs):
    ncores = 8
    x = np.ascontiguousarray(inputs["x"], dtype=np.float32)
    c = np.ascontiguousarray(inputs["c"], dtype=np.float32)
    nc, _ = build_program(nseq=1, nt=16, depth=4)
    wmap = {n: np.ascontiguousarray(inputs[n], dtype=np.float32) for n in WNAMES}
    out = np.empty_like(x)
    for k in range(2):
        in_maps = []
        for i in range(ncores):
            m = dict(wmap)
            m["x"] = x[2 * i + k:2 * i + k + 1]
            m["c"] = c[2 * i + k:2 * i + k + 1]
            in_maps.append(m)
        res = run_bass_kernel_spmd(nc, in_maps, core_ids=list(range(ncores)))
        for i in range(ncores):
            out[2 * i + k] = res.results[i]["out"][0]
    return out
```

```python
import contextlib
import numpy as np
import concourse.bass as bass
import concourse.mybir as mybir
from concourse.bass_utils import run_bass_kernel_spmd

F32 = mybir.dt.float32
BF16 = mybir.dt.bfloat16
AF = mybir.ActivationFunctionType
ALU = mybir.AluOpType
AX = mybir.AxisListType

L_FULL = 4
D = 1024
T = 256
NE = 64
ALPHA = 8.0 ** 0.25
EPS = 1e-5
C_G = 0.7978845608028654
SQ_G = 0.044715 ** 0.5
NSLOT = 4


class Prog:
    ENGS = ("pe", "act", "dve", "pool", "sp")

    def __init__(self):
        self.ops = {e: [] for e in self.ENGS}
        self.res = {}
        self.dma_sem_count = {}

    @staticmethod
    def _canon(names):
        out = []
        for n in names:
            if n.startswith("ps") and n[2:].isdigit():
                n = "pb%d" % (int(n[2:]) // 2)
            if n not in out:
                out.append(n)
        return out

    def op(self, eng, fn, reads=(), writes=(), dma_sem=None, after=()):
        reads = self._canon(reads)
        writes = self._canon(writes)
        idx = len(self.ops[eng])
        me = (eng, idx)
        deps = set(after)
        for r in reads:
            st = self.res.get(r)
            if st is None:
                st = self.res[r] = {"w": None, "r": {}, "rd": []}
            if st["w"] is not None:
                deps.add(st["w"])
        for w in writes:
            st = self.res.get(w)
            if st is None:
                st = self.res[w] = {"w": None, "r": {}, "rd": []}
            if st["w"] is not None:
                deps.add(st["w"])
            for e2, i2 in st["r"].items():
                deps.add((e2, i2))
            for d in st["rd"]:
                deps.add(d)
        if eng == "pe":
            deps = {d for d in deps if d[0] != "pe"}
        deps.discard(me)
        rec = {"fn": fn, "deps": deps, "signal": False, "dma_sem": dma_sem, "tok": None}
        self.ops[eng].append(rec)
        is_dma = dma_sem is not None
        for r in reads:
            st = self.res[r]
            if is_dma:
                st["rd"].append(me)
            else:
                st["r"][eng] = idx
        for w in writes:
            self.res[w] = {"w": me, "r": {}, "rd": []}
        return me

    def finalize(self):
        for e in self.ENGS:
            for rec in self.ops[e]:
                for (de, di) in rec["deps"]:
                    self.ops[de][di]["signal"] = True
        for e in self.ENGS:
            cnt = 0
            for rec in self.ops[e]:
                if rec["dma_sem"] is not None:
                    s = rec["dma_sem"]
                    self.dma_sem_count[s] = self.dma_sem_count.get(s, 0) + 16
                    rec["tok"] = (s, self.dma_sem_count[s])
                    rec["signal"] = True
                elif rec["signal"]:
                    cnt += 1
                    rec["tok"] = (e, cnt)

    def emit(self, eng_name, eng, sems):
        waited = {}
        for rec in self.ops[eng_name]:
            need = {}
            for (de, di) in rec["deps"]:
                s, v = self.ops[de][di]["tok"]
                if need.get(s, 0) < v:
                    need[s] = v
            for s, v in need.items():
                if waited.get(s, 0) >= v:
                    continue
                eng.wait_ge(sems[s], v)
                waited[s] = v
            inst = rec["fn"](eng)
            if rec["signal"]:
                s, v = rec["tok"]
                inst.then_inc(sems[s], 16 if rec["dma_sem"] is not None else 1)


WNAMES = ["ada_w", "ada_b", "w_in", "sgu_ln_g", "sgu_ln_b", "sgu_w", "sgu_b", "conv_w", "conv_b",
          "rg_wa", "rg_ba", "rg_wx", "rg_bx", "rg_lambda", "w_branch_a", "w_branch_b", "w_out",
          "ln1_g", "ln1_b", "router_w", "router_b", "exp_w1", "exp_w3", "exp_w2", "sh_w1", "sh_w3",
          "sh_w2", "ln2_g", "ln2_b"]
WSHAPES = {
    "ada_w": [4, 1024, 6144], "ada_b": [4, 6144], "w_in": [4, 1024, 6144], "sgu_ln_g": [4, 1024],
    "sgu_ln_b": [4, 1024], "sgu_w": [4, 8, 128, 128], "sgu_b": [4, 8, 128], "conv_w": [4, 4, 1024],
    "conv_b": [4, 1024], "rg_wa": [4, 8, 128, 128], "rg_ba": [4, 1024], "rg_wx": [4, 8, 128, 128],
    "rg_bx": [4, 1024], "rg_lambda": [4, 1024], "w_branch_a": [4, 1024, 1024],
    "w_branch_b": [4, 1024, 1024], "w_out": [4, 1024, 1024], "ln1_g": [4, 1024], "ln1_b": [4, 1024],
    "router_w": [4, 1024, 64], "router_b": [4, 64], "exp_w1": [4, 64, 1024, 256],
    "exp_w3": [4, 64, 1024, 256], "exp_w2": [4, 64, 256, 1024], "sh_w1": [4, 1024, 256],
    "sh_w3": [4, 1024, 256], "sh_w2": [4, 256, 1024], "ln2_g": [4, 1024], "ln2_b": [4, 1024],
}
PV = ["sgu_ln_g", "sgu_ln_b", "conv_b", "rg_ba", "rg_bx", "rg_lambda", "ln1_g", "ln1_b", "ln2_g", "ln2_b",
      "cw0", "cw1", "cw2", "cw3", "ab0", "ab1", "ab2", "ab3", "ab4", "ab5"]


def build_program(nseq=2, nt=16, depth=4, seq=4096, dbg=None, phases="ABC", nlayers_cast=None):
    nc = bass.Bass("TRN2", target_bir_lowering=False)
    P = Prog()
    LD = depth
    x_d = nc.dram_tensor("x", [nseq, seq, D], F32, kind="ExternalInput").ap()
    c_d = nc.dram_tensor("c", [nseq, D], F32, kind="ExternalInput").ap()
    W = {n: nc.dram_tensor(n, WSHAPES[n], F32, kind="ExternalInput").ap() for n in WNAMES}
    out_d = nc.dram_tensor("out", [nseq, nt * T, D], F32, kind="ExternalOutput").ap()
    wmix_d = [nc.dram_tensor("wmix%d" % l, [18, 128, 4096], BF16, kind="Internal").ap() for l in range(LD)]
    wexp_d = [nc.dram_tensor("wexp%d" % l, [NE + 1, 128, 6144], BF16, kind="Internal").ap() for l in range(LD)]
    dbg_d = {}
    if dbg:
        for name, shape in dbg.items():
            dbg_d[name] = nc.dram_tensor("dbg_" + name, shape, F32, kind="ExternalOutput").ap()

    es = contextlib.ExitStack()
    with es:
        def sb(name, shape, dt=F32):
            return es.enter_context(nc.sbuf_tensor(name, shape, dt))

        ident = sb("ident", [128, 128]); onesD = sb("onesD", [128, 128]); onesb = sb("onesb", [128, 128], BF16)
        identb = sb("identb", [128, 128], BF16); tril = sb("tril", [128, 128])
        stgp = sb("stgp", [128, 6, 128]); prm = sb("prm", [128, 6, 128])
        adaS = sb("adaS", [128, LD, 6, 8, nseq]); dv = sb("dv", [128, LD, nseq, 6, 8])
        cact = sb("cact", [128, 16])
        spm = sb("spm", [128, LD, 4, 8])
        WsT = sb("WsT", [128, LD, 8, 128], BF16); bias2 = sb("bias2", [128, LD, 8, 128])
        rgw = sb("rgw", [128, LD, 2, 8, 128], BF16); wr = sb("wr", [128, LD, 8, NE], BF16)
        rb = sb("rb", [128, LD, NE])
        ring = sb("ring", [128, NSLOT, 6144], BF16)
        xT = sb("xT", [128, 8, T]); hT = sb("hT", [128, 8, T], BF16)
        xtok = sb("xtok", [128, 2, D]); scr = sb("scr", [128, 8192])
        BA = sb("BA", [128, 8, T], BF16); BB = sb("BB", [128, 8, T], BF16)
        rin = sb("rin", [128, 8, T + 3])
        mS = sb("mS", [128, T]); vS = sb("vS", [128, T]); rS = sb("rS", [128, T])
        st6 = sb("st6", [128, 2, 2, 6]); mv = sb("mv", [128, 2, 2]); rstd = sb("rstd", [128, 2])
        r_sc = sb("r_sc", [128, 2, NE]); r_bi = sb("r_bi", [128, 2, NE]); r_t = sb("r_t", [128, 2, NE])
        r_m1 = sb("r_m1", [128, 2, 8]); r_m2 = sb("r_m2", [128, 2, 8]); r_k = sb("r_k", [128, 2, 8])
        r_s8 = sb("r_s8", [128, 2, 8]); r_mb = sb("r_mb", [128, 2, NE]); r_e8 = sb("r_e8", [128, 2, 8])
        r_w = sb("r_w", [128, 2, NE]); r_d = sb("r_d", [128, 2]); r_G = sb("r_G", [128, 2, NE])
        GT = sb("GT", [128, T], BF16)
        tbq = [sb("tb0", [128, 256]), sb("tb1", [128, 256])]
        htok = [sb("ht0", [128, 256], BF16), sb("ht1", [128, 256], BF16)]
        hx = [sb("hx%d" % i, [128, 2, 128], BF16) for i in range(3)]
        convh = sb("convh", [128, LD, 8, 3]); hst = sb("hst", [128, LD, 8])
        ps = es.enter_context(nc.psum_tensor("ps", [128, 4096], F32))

        sem_names = list(Prog.ENGS) + ["wl%d" % k for k in range(NSLOT)] + ["ws%d" % k for k in range(NSLOT)] + \
            ["sg%d" % k for k in range(4)] + ["pl", "xl", "st", "dbg"]
        sems = {s: es.enter_context(nc.semaphore(s)) for s in sem_names}

        Fv = [scr[:, i * 2048:(i + 1) * 2048].rearrange("p (m t) -> p m t", t=T) for i in range(4)]
        Fn = ["FA", "FB", "FC", "FD"]
        gB = scr[:].bitcast(BF16).rearrange("p (e t) -> p e t", t=T)
        vn = BB[:].rearrange("p m t -> p (m t)").rearrange("p (s c) -> p s c", s=2)
        STG = [scr[:, i * 2048:(i + 1) * 2048] for i in range(4)]

        tpv = [ps[:, (2 + k) * 512:(3 + k) * 512].bitcast(BF16) for k in range(2)]

        def psh(i):
            return ps[:, i * 256:(i + 1) * 256]

        def psb(b):
            return ps[:, b * 512:(b + 1) * 512]

        def psn(lo, hi):
            return ["ps%d" % i for i in range(lo, hi)]

        def fres(k, banks=range(4)):
            return ["%s%d" % (Fn[k], b) for b in banks]

        def prmv(v, l):
            return prm[:, v // 4, (v % 4) * 32 + l * 8:(v % 4) * 32 + l * 8 + 8]

        def pv(name, l):
            return prmv(PV.index(name), l)

        def bc_t(ap8):
            return ap8.unsqueeze(2).to_broadcast([128, 8, T])

        P.op("pool", lambda e: e.memset(ident[:], 1.0), writes=["ident"])
        P.op("pool", lambda e: e.affine_select(out=ident[:], in_=ident[:], pattern=[[-1, 128]], compare_op=ALU.is_equal, fill=0.0, base=0, channel_multiplier=1), reads=["ident"], writes=["ident"])
        P.op("pool", lambda e: e.memset(tril[:], 1.0), writes=["tril"])
        P.op("pool", lambda e: e.affine_select(out=tril[:], in_=tril[:], pattern=[[-1, 128]], compare_op=ALU.is_ge, fill=0.0, base=0, channel_multiplier=1), reads=["tril"], writes=["tril"])
        P.op("pool", lambda e: e.memset(onesD[:], 1.0 / D), writes=["onesD"])
        P.op("pool", lambda e: e.memset(onesb[:], 1.0), writes=["onesb"])
        P.op("pool", lambda e: e.tensor_copy(out=identb[:], in_=ident[:]), reads=["ident"], writes=["identb"])
        P.op("pool", lambda e: e.memset(stgp[:], 0.0), writes=["stgp"])

        pl_ops = []
        ms = P.ops["pool"]
        stgp_init = ("pool", len(ms) - 1)

        def pload(v, l, src):
            b, o = v // 4, (v % 4) * 32 + l * 8
            pl_ops.append(P.op("sp", lambda e: e.dma_start(out=stgp[o:o + 8, b, :], in_=src), dma_sem="pl", after=[stgp_init]))
        for l in range(LD):
            for v, name in enumerate(PV):
                if name.startswith("cw"):
                    src = W["conv_w"][l, int(name[2]), :].rearrange("(j p) -> j p", p=128)
                elif name.startswith("ab"):
                    k = int(name[2])
                    src = W["ada_b"][l, k * D:(k + 1) * D].rearrange("(j p) -> j p", p=128)
                else:
                    src = W[name][l, :].rearrange("(j p) -> j p", p=128)
                pload(v, l, src)
        pl_ops.append(P.op("sp", lambda e: e.dma_start(out=stgp[0:nseq * 8, 5, :], in_=c_d.rearrange("s (j p) -> (s j) p", p=128)), dma_sem="pl", after=[stgp_init]))
        for b in range(6):
            P.op("pe", lambda e, b=b: e.transpose(out=psh(b)[:, 0:128], in_=stgp[:, b, :], identity=ident[:]), reads=["ident"], writes=["ps%d" % b], after=pl_ops)
            P.op("dve", lambda e, b=b: e.tensor_copy(out=prm[:, b, :], in_=psh(b)[:, 0:128]), reads=["ps%d" % b], writes=["prm"])
        ncol = nseq * 8
        P.op("act", lambda e: e.activation(out=cact[:, 0:ncol], in_=prm[:, 5, 0:ncol], func=AF.Tanh, scale=0.5), reads=["prm"], writes=["cact"])
        P.op("dve", lambda e: e.scalar_tensor_tensor(out=cact[:, 0:ncol], in0=cact[:, 0:ncol], scalar=1.0, in1=prm[:, 5, 0:ncol], op0=ALU.add, op1=ALU.mult), reads=["cact", "prm"], writes=["cact"])
        P.op("dve", lambda e: e.tensor_scalar(out=cact[:, 0:ncol], in0=cact[:, 0:ncol], scalar1=0.5, scalar2=None, op0=ALU.mult), reads=["cact"], writes=["cact"])
        for l in range(LD):
            P.op("act", lambda e, l=l: e.activation(out=spm[:, l, 0, :], in_=pv("rg_lambda", l), func=AF.Exp, scale=-1.0), reads=["prm"], writes=["spm"])
        for l in range(LD):
            P.op("act", lambda e, l=l: e.activation(out=spm[:, l, 0, :], in_=spm[:, l, 0, :], func=AF.Ln, bias=1.0), reads=["spm"], writes=["spm"])
        for l in range(LD):
            P.op("dve", lambda e, l=l: e.tensor_scalar(out=spm[:, l, 1, :], in0=spm[:, l, 0, :], scalar1=-8.0, scalar2=None, op0=ALU.mult), reads=["spm"], writes=["spm"])
            P.op("dve", lambda e, l=l: e.tensor_scalar(out=spm[:, l, 0, :], in0=spm[:, l, 0, :], scalar1=-4.0, scalar2=None, op0=ALU.mult), reads=["spm"], writes=["spm"])
            P.op("dve", lambda e, l=l: e.tensor_scalar(out=spm[:, l, 2, :], in0=pv("rg_ba", l), scalar1=0.5, scalar2=None, op0=ALU.mult), reads=["prm"], writes=["spm"])
            P.op("dve", lambda e, l=l: e.tensor_scalar(out=spm[:, l, 3, :], in0=pv("rg_bx", l), scalar1=0.5, scalar2=None, op0=ALU.mult), reads=["prm"], writes=["spm"])

        adaP = ps[:, 3072:3072 + LD * 48 * nseq].rearrange("p (l v m s) -> p l v m s", l=LD, v=6, m=8)
        adaPn = psn(12, 16)
        sgi = 0
        import os as _os
        _pha = _os.environ.get('PHA', '123')
        for l in (range(LD) if '2' in _pha else []):
            for q in range(24):
                k = sgi % 4
                sgi += 1
                stg = STG[k].rearrange("p (j n) -> p j n", n=256)
                src = W["ada_w"][l, :, q * 256:(q + 1) * 256].rearrange("(j p) n -> p j n", p=128)
                P.op("sp", lambda e, stg=stg, src=src: e.dma_start(out=stg, in_=src), writes=["stg%d" % k], dma_sem="sg%d" % k)
                for mm in range(2):
                    v, m = (q * 2 + mm) // 8, (q * 2 + mm) % 8
                    for j in range(8):
                        P.op("pe", lambda e, stg=stg, l=l, v=v, m=m, j=j, mm=mm: e.matmul(
                            adaP[:, l, v, m, :], lhsT=stg[:, j, mm * 128:(mm + 1) * 128],
                            rhs=(cact[:, j:j + 9:8] if nseq == 2 else cact[:, j:j + 1]), start=(j == 0), stop=(j == 7)),
                            reads=["stg%d" % k, "cact"], writes=adaPn)
        for l in (range(LD) if '2' in _pha else []):
            for k6 in range(6):
                vv = 14 + k6
                P.op("dve", lambda e, l=l, k6=k6, vv=vv: e.tensor_tensor(
                    out=adaS[:, l, k6, :, :], in0=adaP[:, l, k6, :, :],
                    in1=prmv(vv, l).unsqueeze(2).to_broadcast([128, 8, nseq]), op=ALU.add),
                    reads=adaPn + ["prm"], writes=["adaS"])
        for l in (range(LD) if '2' in _pha else []):
            for s in range(nseq):
                A = lambda k6, l=l, s=s: adaS[:, l, k6, :, s]
                o = lambda k, l=l, s=s: dv[:, l, s, k, :]
                P.op("dve", lambda e, A=A, o=o: e.tensor_scalar(out=o(0), in0=A(1), scalar1=1.0, scalar2=None, op0=ALU.add), reads=["adaS"], writes=["dv"])
                P.op("dve", lambda e, A=A, o=o: e.tensor_copy(out=o(1), in_=A(0)), reads=["adaS"], writes=["dv"])
                P.op("dve", lambda e, A=A, o=o: e.tensor_scalar(out=o(2), in0=A(2), scalar1=1.0 / (8.0 * ALPHA), scalar2=None, op0=ALU.mult), reads=["adaS"], writes=["dv"])
                P.op("dve", lambda e, A=A, o=o: e.tensor_scalar(out=o(5), in0=A(4), scalar1=1.0, scalar2=None, op0=ALU.add), reads=["adaS"], writes=["dv"])
                P.op("dve", lambda e, A=A, o=o, l=l: e.tensor_tensor(out=o(3), in0=o(5), in1=pv("ln1_g", l), op=ALU.mult), reads=["dv", "prm"], writes=["dv"])
                P.op("dve", lambda e, A=A, o=o, l=l: e.tensor_tensor(out=o(4), in0=o(5), in1=pv("ln1_b", l), op=ALU.mult), reads=["dv", "prm"], writes=["dv"])
                P.op("dve", lambda e, A=A, o=o: e.tensor_tensor(out=o(4), in0=o(4), in1=A(3), op=ALU.add), reads=["dv", "adaS"], writes=["dv"])
                P.op("dve", lambda e, A=A, o=o: e.tensor_scalar(out=o(5), in0=A(5), scalar1=1.0 / ALPHA, scalar2=None, op0=ALU.mult), reads=["adaS", "dv"], writes=["dv"])

        for l in (range(LD) if '3' in _pha else []):
            k = sgi % 4
            sgi += 1
            stg = STG[k][:, 0:1024].rearrange("p (g s) -> p g s", s=128)
            P.op("sp", lambda e, stg=stg, l=l: e.dma_start(out=stg, in_=W["sgu_w"][l].rearrange("g t s -> t g s")), writes=["stg%d" % k], dma_sem="sg%d" % k)
            P.op("pool", lambda e, stg=stg: e.tensor_tensor(out=stg, in0=stg, in1=tril[:].unsqueeze(1).to_broadcast([128, 8, 128]), op=ALU.mult), reads=["stg%d" % k, "tril"], writes=["stg%d" % k])
            for g in range(8):
                P.op("pe", lambda e, stg=stg, g=g: e.transpose(out=psh(g)[:, 0:128], in_=stg[:, g, :], identity=ident[:]), reads=["stg%d" % k, "ident"], writes=["ps%d" % g])
                P.op("act", lambda e, l=l, g=g: e.copy(out=WsT[:, l, g, :], in_=psh(g)[:, 0:128]), reads=["ps%d" % g], writes=["WsT"])
            k2 = sgi % 4
            sgi += 1
            stg2 = STG[k2][:, 0:1024]
            P.op("sp", lambda e, stg2=stg2, l=l: e.dma_start(out=stg2, in_=W["sgu_b"][l:l + 1].rearrange("o g t -> o (g t)").to_broadcast([128, 1024])), writes=["stg%d" % k2], dma_sem="sg%d" % k2)
            for g in range(8):
                P.op("pe", lambda e, l=l, g=g: e.matmul(psh(8 + g)[:, 0:128], lhsT=onesb[:], rhs=WsT[:, l, g, :], start=True, stop=True), reads=["onesb", "WsT"], writes=["ps%d" % (8 + g)])
                P.op("dve", lambda e, l=l, g=g, stg2=stg2: e.scalar_tensor_tensor(
                    out=bias2[:, l, g, :], in0=psh(8 + g)[:, 0:128], scalar=pv("sgu_ln_b", l)[:, g:g + 1],
                    in1=stg2[:, g * 128:(g + 1) * 128], op0=ALU.mult, op1=ALU.add),
                    reads=["ps%d" % (8 + g), "prm", "stg%d" % k2], writes=["bias2"])
            for wi, wn in enumerate(["rg_wa", "rg_wx"]):
                k3 = sgi % 4
                sgi += 1
                stg3 = STG[k3][:, 0:1024].rearrange("p (h j) -> p h j", j=128)
                P.op("sp", lambda e, stg3=stg3, l=l, wn=wn: e.dma_start(out=stg3, in_=W[wn][l].rearrange("h i j -> i h j")), writes=["stg%d" % k3], dma_sem="sg%d" % k3)
                P.op("dve", lambda e, stg3=stg3, l=l, wi=wi: e.tensor_copy(out=rgw[:, l, wi, :, :], in_=stg3), reads=["stg%d" % k3], writes=["rgw"])
            k4 = sgi % 4
            sgi += 1
            stg4 = STG[k4][:, 0:512].rearrange("p (j n) -> p j n", n=NE)
            P.op("sp", lambda e, stg4=stg4, l=l: e.dma_start(out=stg4, in_=W["router_w"][l].rearrange("(j p) n -> p j n", p=128)), writes=["stg%d" % k4], dma_sem="sg%d" % k4)
            P.op("dve", lambda e, stg4=stg4, l=l: e.tensor_copy(out=wr[:, l, :, :], in_=stg4), reads=["stg%d" % k4], writes=["wr"])
            P.op("sp", lambda e, l=l: e.dma_start(out=rb[:, l, :], in_=W["router_b"][l:l + 1, :].to_broadcast([128, NE])), writes=["rb"], dma_sem="pl")

        wstores = []
        first_wload = [True]
        MIXW = [("w_in", 1), ("w_in", 0), ("w_in", 3), ("w_in", 2), ("w_in", 4), ("w_in", 5),
                ("w_branch_a", None), ("w_branch_b", None), ("w_out", None)]
        cast_i = [0]
        slot_i = [0]

        sgi_box = [sgi]
        for l in (range(LD) if "B" in phases else []):
            for ci, (wn, slot_w) in enumerate(MIXW):
                for half in range(2):
                    slot = slot_i[0] % NSLOT
                    slot_i[0] += 1
                    for q in range(2):
                        c0 = half * 512 + q * 256
                        if wn == "w_in":
                            src = W[wn][l, :, slot_w * 1024 + c0: slot_w * 1024 + c0 + 256]
                        else:
                            src = W[wn][l, :, c0:c0 + 256]
                        src = src.rearrange("(j p) n -> p j n", p=128)
                        k = sgi_box[0] % 4
                        sgi_box[0] += 1
                        stgv = STG[k].rearrange("p (j n) -> p j n", n=256)
                        P.op("sp", lambda e, stgv=stgv, src=src: e.dma_start(out=stgv, in_=src), writes=["stg%d" % k], dma_sem="sg%d" % k)
                        dst = ring[:, slot, 0:4096].rearrange("p (j n) -> p j n", n=512)[:, :, q * 256:(q + 1) * 256]
                        eng = "act" if cast_i[0] % 2 == 0 else "dve"
                        cast_i[0] += 1
                        if eng == "act":
                            P.op("act", lambda e, dst=dst, stgv=stgv: e.copy(out=dst, in_=stgv), reads=["stg%d" % k], writes=["ring%d" % slot])
                        else:
                            P.op("dve", lambda e, dst=dst, stgv=stgv: e.tensor_copy(out=dst, in_=stgv), reads=["stg%d" % k], writes=["ring%d" % slot])
                    wstores.append(P.op("sp", lambda e, slot=slot, l=l, ci=ci, half=half: e.dma_start(out=wmix_d[l][ci * 2 + half], in_=ring[:, slot, 0:4096]),
                         reads=["ring%d" % slot], dma_sem="ws%d" % slot))
            for ex in range(NE + 1):
                slot = slot_i[0] % NSLOT
                slot_i[0] += 1
                if ex < NE:
                    s1, s3, s2 = W["exp_w1"][l, ex], W["exp_w3"][l, ex], W["exp_w2"][l, ex]
                else:
                    s1, s3, s2 = W["sh_w1"][l], W["sh_w3"][l], W["sh_w2"][l]
                for q, src in enumerate([s1, s3]):
                    src = src.rearrange("(j p) n -> p j n", p=128)
                    k = sgi_box[0] % 4
                    sgi_box[0] += 1
                    stgv = STG[k].rearrange("p (j n) -> p j n", n=256)
                    P.op("sp", lambda e, stgv=stgv, src=src: e.dma_start(out=stgv, in_=src), writes=["stg%d" % k], dma_sem="sg%d" % k)
                    dst = ring[:, slot, 0:4096].rearrange("p (j n) -> p j n", n=512)[:, :, q * 256:(q + 1) * 256]
                    eng = "act" if cast_i[0] % 2 == 0 else "dve"
                    cast_i[0] += 1
                    if eng == "act":
                        P.op("act", lambda e, dst=dst, stgv=stgv: e.copy(out=dst, in_=stgv), reads=["stg%d" % k], writes=["ring%d" % slot])
                    else:
                        P.op("dve", lambda e, dst=dst, stgv=stgv: e.tensor_copy(out=dst, in_=stgv), reads=["stg%d" % k], writes=["ring%d" % slot])
                src = s2.rearrange("(f p) o -> p f o", p=128)
                k = sgi_box[0] % 4
                sgi_box[0] += 1
                stgv = STG[k].rearrange("p (f o) -> p f o", f=2)
                P.op("sp", lambda e, stgv=stgv, src=src: e.dma_start(out=stgv, in_=src), writes=["stg%d" % k], dma_sem="sg%d" % k)
                dst = ring[:, slot, 4096:6144].rearrange("p (f o) -> p f o", f=2)
                eng = "act" if cast_i[0] % 2 == 0 else "dve"
                cast_i[0] += 1
                if eng == "act":
                    P.op("act", lambda e, dst=dst, stgv=stgv: e.copy(out=dst, in_=stgv), reads=["stg%d" % k], writes=["ring%d" % slot])
                else:
                    P.op("dve", lambda e, dst=dst, stgv=stgv: e.tensor_copy(out=dst, in_=stgv), reads=["stg%d" % k], writes=["ring%d" % slot])
                wstores.append(P.op("sp", lambda e, slot=slot, l=l, ex=ex: e.dma_start(out=wexp_d[l][ex], in_=ring[:, slot, :]),
                     reads=["ring%d" % slot], dma_sem="ws%d" % slot))

        def wload(src, width):
            slot = slot_i[0] % NSLOT
            slot_i[0] += 1
            aft = wstores if first_wload[0] else ()
            first_wload[0] = False
            P.op("sp", lambda e: e.dma_start(out=ring[:, slot, 0:width], in_=src),
                 writes=["ring%d" % slot], dma_sem="wl%d" % slot, after=aft)
            return slot

        def gelu2(src, dst, rsrc, rdst):
            P.op("act", lambda e: e.activation(out=dst, in_=src, func=AF.Square, scale=SQ_G), reads=rsrc, writes=rdst)
            P.op("dve", lambda e: e.scalar_tensor_tensor(out=dst, in0=dst, scalar=1.0, in1=src, op0=ALU.add, op1=ALU.mult), reads=rsrc + rdst, writes=rdst)
            P.op("act", lambda e: e.activation(out=dst, in_=dst, func=AF.Tanh, scale=C_G), reads=rdst, writes=rdst)
            P.op("dve", lambda e: e.scalar_tensor_tensor(out=dst, in0=dst, scalar=1.0, in1=src, op0=ALU.add, op1=ALU.mult), reads=rsrc + rdst, writes=rdst)

        def fm_matmul(l, ci, rhs_ap, rhs_res, ps_base):
            for half in range(2):
                slot = wload(wmix_d[l][ci * 2 + half], 4096)
                wv = ring[:, slot, 0:4096].rearrange("p (j n) -> p j n", n=512)
                for mm in range(4):
                    m = half * 4 + mm
                    for j in range(8):
                        P.op("pe", lambda e, wv=wv, mm=mm, m=m, j=j: e.matmul(
                            psh(ps_base + m), lhsT=wv[:, j, mm * 128:(mm + 1) * 128], rhs=rhs_ap[:, j, :],
                            start=(j == 0), stop=(j == 7)),
                            reads=["ring%d" % slot] + rhs_res, writes=["ps%d" % (ps_base + m)])

        def layer_norm_fm(res, res_names, sq, sq_names, eps):
            for b in range(4):
                P.op("act", lambda e, b=b: e.activation(out=sq[:, 2 * b:2 * b + 2, :], in_=res[:, 2 * b:2 * b + 2, :], func=AF.Square),
                     reads=[res_names[b]], writes=[sq_names[b]])
            for j in range(8):
                P.op("pe", lambda e, j=j: e.matmul(psh(0), lhsT=onesD[:], rhs=res[:, j, :], start=(j == 0), stop=(j == 7)),
                     reads=["onesD", res_names[j // 2]], writes=["ps0"])
            for j in range(8):
                P.op("pe", lambda e, j=j: e.matmul(psh(1), lhsT=onesD[:], rhs=sq[:, j, :], start=(j == 0), stop=(j == 7)),
                     reads=["onesD", sq_names[j // 2]], writes=["ps1"])
            P.op("act", lambda e: e.copy(out=mS[:], in_=psh(0)), reads=["ps0"], writes=["mS"])
            P.op("dve", lambda e: e.tensor_tensor(out=vS[:], in0=mS[:], in1=psh(0), op=ALU.mult), reads=["mS", "ps0"], writes=["vS"])
            P.op("dve", lambda e: e.scalar_tensor_tensor(out=vS[:], in0=vS[:], scalar=-1.0, in1=psh(1), op0=ALU.mult, op1=ALU.add), reads=["vS", "ps1"], writes=["vS"])
            P.op("act", lambda e: e.activation(out=rS[:], in_=vS[:], func=AF.Sqrt, bias=eps), reads=["vS"], writes=["rS"])
            P.op("dve", lambda e: e.reciprocal(out=rS[:], in_=rS[:]), reads=["rS"], writes=["rS"])
            for b in range(4):
                P.op("pool", lambda e, b=b: e.tensor_tensor(out=res[:, 2 * b:2 * b + 2, :], in0=res[:, 2 * b:2 * b + 2, :],
                     in1=mS[:].unsqueeze(1).to_broadcast([128, 2, T]), op=ALU.subtract), reads=[res_names[b], "mS"], writes=[res_names[b]])
                P.op("pool", lambda e, b=b: e.tensor_tensor(out=res[:, 2 * b:2 * b + 2, :], in0=res[:, 2 * b:2 * b + 2, :],
                     in1=rS[:].unsqueeze(1).to_broadcast([128, 2, T]), op=ALU.mult), reads=[res_names[b], "rS"], writes=[res_names[b]])

        def affine_fm(src, src_names, tmp, tmp_names, dst, dst_name, g8, b8, pres):
            P.op("dve", lambda e: e.tensor_tensor(out=tmp, in0=src, in1=bc_t(g8), op=ALU.mult), reads=src_names + pres, writes=tmp_names)
            P.op("pool", lambda e: e.tensor_tensor(out=dst, in0=tmp, in1=bc_t(b8), op=ALU.add), reads=tmp_names + pres, writes=[dst_name])

        def dump(name, ap, res):
            if name in dbg_d:
                P.op("sp", lambda e: e.dma_start(out=dbg_d[name], in_=ap), reads=res, dma_sem="dbg")

        FA, FB, FC, FD = Fv
        FA4 = scr[:, 0:2048].rearrange("p (m s t) -> p m s t", m=8, s=2)

        for s in (range(nseq) if "C" in phases else []):
            P.op("pool", lambda e: e.memset(convh[:], 0.0), reads=["convh"], writes=["convh"])
            P.op("pool", lambda e: e.memset(hst[:], 0.0), reads=["hst"], writes=["hst"])
            for it in range(nt):
                t0 = it * T
                P.op("sp", lambda e, s=s, t0=t0: e.dma_start(out=xtok[:], in_=x_d[s, t0:t0 + T, :].rearrange("(u p) d -> p u d", p=128)),
                     writes=["vg0", "vg1", "vg2", "vg3"], dma_sem="xl")
                for m in range(8):
                    for u in range(2):
                        P.op("pe", lambda e, m=m, u=u: e.transpose(out=psh(m)[:, u * 128:(u + 1) * 128], in_=xtok[:, u, m * 128:(m + 1) * 128], identity=ident[:]),
                             reads=["vg%d" % (u * 2 + m // 4), "ident"], writes=["ps%d" % m])
                for b in range(4):
                    P.op("act", lambda e, b=b: e.copy(out=xT[:, 2 * b:2 * b + 2, :], in_=psb(b).rearrange("p (m t) -> p m t", t=T)),
                         reads=psn(2 * b, 2 * b + 2), writes=["xT"])
                for l in range(LD):
                    d = lambda k, l=l, s=s: dv[:, l, s, k, :]
                    affine_fm(xT[:], ["xT"], FD, fres(3), hT[:], "hT", d(0), d(1), ["dv"])
                    for half in range(2):
                        slot = wload(wmix_d[l][0 * 2 + half], 4096)
                        wv = ring[:, slot, 0:4096].rearrange("p (j n) -> p j n", n=512)
                        for u in range(2):
                            for j in range(8):
                                P.op("pe", lambda e, wv=wv, u=u, j=j, half=half: e.matmul(
                                    psb(u * 2 + half), lhsT=hT[:, j, u * 128:(u + 1) * 128], rhs=wv[:, j, :], start=(j == 0), stop=(j == 7)),
                                    reads=["ring%d" % slot, "hT"], writes=psn(2 * (u * 2 + half), 2 * (u * 2 + half) + 2))
                    for u in range(2):
                        for half in range(2):
                            b = u * 2 + half
                            gelu2(psb(b), xtok[:, u, half * 512:(half + 1) * 512], psn(2 * b, 2 * b + 2), ["vg%d" % b])
                            P.op("dve", lambda e, u=u, half=half: e.bn_stats(out=st6[:, u, half, :], in_=xtok[:, u, half * 512:(half + 1) * 512]),
                                 reads=["vg%d" % b], writes=["st6_%d" % b])
                        P.op("dve", lambda e, u=u: e.bn_aggr(out=mv[:, u, :], in_=st6[:, u, :, :].rearrange("p a b -> p (a b)")),
                             reads=["st6_%d" % (u * 2), "st6_%d" % (u * 2 + 1)], writes=["mv%d" % u])
                    P.op("act", lambda e: e.activation(out=rstd[:], in_=mv[:, :, 1], func=AF.Sqrt, bias=4.0 * EPS), reads=["mv0", "mv1"], writes=["rstd"])
                    P.op("dve", lambda e: e.reciprocal(out=rstd[:], in_=rstd[:]), reads=["rstd"], writes=["rstd"])
                    for u in range(2):
                        P.op("dve", lambda e, u=u: e.tensor_scalar(out=vn[:, u, :], in0=xtok[:, u, :], scalar1=mv[:, u, 0:1], scalar2=rstd[:, u:u + 1],
                             op0=ALU.subtract, op1=ALU.mult), reads=["vg%d" % (2 * u), "vg%d" % (2 * u + 1), "mv%d" % u, "rstd"], writes=["BB%d" % (2 * u), "BB%d" % (2 * u + 1)])
                    for g in range(8):
                        for u in range(2):
                            P.op("pe", lambda e, g=g, u=u, l=l: e.matmul(psh(8 + g)[:, u * 128:(u + 1) * 128], lhsT=vn[:, u, g * 128:(g + 1) * 128],
                                 rhs=WsT[:, l, g, :], start=True, stop=True), reads=["BB%d" % (2 * u + g // 4), "WsT"], writes=["ps%d" % (8 + g)])
                        P.op("dve", lambda e, g=g, l=l: e.scalar_tensor_tensor(
                            out=FA4[:, g, :, :], in0=psh(8 + g).rearrange("p (s t) -> p s t", s=2), scalar=pv("sgu_ln_g", l)[:, g:g + 1],
                            in1=bias2[:, l, g, :].unsqueeze(1).to_broadcast([128, 2, 128]), op0=ALU.mult, op1=ALU.add),
                            reads=["ps%d" % (8 + g), "prm", "bias2"], writes=["FA%d" % (g // 2)])
                    fm_matmul(l, 1, hT, ["hT"], 0)
                    for b in range(4):
                        gelu2(psb(b), FB[:, 2 * b:2 * b + 2, :].rearrange("p m t -> p (m t)"), psn(2 * b, 2 * b + 2), ["FB%d" % b])
                        P.op("pool", lambda e, b=b: e.tensor_tensor(out=BA[:, 2 * b:2 * b + 2, :], in0=FB[:, 2 * b:2 * b + 2, :], in1=FA[:, 2 * b:2 * b + 2, :], op=ALU.mult),
                             reads=["FB%d" % b, "FA%d" % b], writes=["BA%d" % b])
                    fm_matmul(l, 2, hT, ["hT"], 8)
                    P.op("pool", lambda e, l=l: e.tensor_copy(out=rin[:, :, 0:3], in_=convh[:, l, :, :]), reads=["convh"], writes=["rinh"])
                    for b in range(4):
                        P.op("act", lambda e, b=b: e.copy(out=rin[:, 2 * b:2 * b + 2, 3:3 + T], in_=psb(4 + b).rearrange("p (m t) -> p m t", t=T)),
                             reads=psn(8 + 2 * b, 10 + 2 * b), writes=["rin%d" % b])
                    rin_all = ["rinh"] + ["rin%d" % b for b in range(4)]
                    P.op("pool", lambda e, l=l: e.tensor_copy(out=convh[:, l, :, :], in_=rin[:, :, T:T + 3]), reads=rin_all + ["convh"], writes=["convh"])
                    P.op("pool", lambda e, l=l: e.tensor_tensor(out=FB, in0=rin[:, :, 0:T], in1=bc_t(pv("cw0", l)), op=ALU.mult), reads=rin_all + ["prm"], writes=fres(1))
                    P.op("pool", lambda e, l=l: e.tensor_tensor(out=FB, in0=FB, in1=bc_t(pv("conv_b", l)), op=ALU.add), reads=fres(1) + ["prm"], writes=fres(1))
                    for k in range(1, 4):
                        P.op("pool", lambda e, l=l, k=k: e.tensor_tensor(out=FC, in0=rin[:, :, k:k + T], in1=bc_t(pv("cw%d" % k, l)), op=ALU.mult), reads=rin_all + ["prm"], writes=fres(2))
                        P.op("pool", lambda e: e.tensor_tensor(out=FB, in0=FB, in1=FC, op=ALU.add), reads=fres(1) + fres(2), writes=fres(1))
                    P.op("act", lambda e: e.copy(out=BB[:], in_=FB), reads=fres(1), writes=["BB%d" % b for b in range(4)])
                    for wi in range(2):
                        for h in range(8):
                            P.op("pe", lambda e, l=l, wi=wi, h=h: e.matmul(psh(wi * 8 + h), lhsT=rgw[:, l, wi, h, :], rhs=BB[:, h, :], start=True, stop=True),
                                 reads=["rgw", "BB%d" % (h // 2)], writes=["ps%d" % (wi * 8 + h)])
                    for h in range(8):
                        P.op("act", lambda e, l=l, h=h: e.activation(out=FC[:, h, :], in_=psh(h), func=AF.Tanh, scale=0.5, bias=spm[:, l, 2, h:h + 1]),
                             reads=["ps%d" % h, "spm"], writes=["FC%d" % (h // 2)])
                        P.op("act", lambda e, l=l, h=h: e.activation(out=FD[:, h, :], in_=FC[:, h, :], func=AF.Exp, scale=spm[:, l, 0, h:h + 1], bias=spm[:, l, 0, h:h + 1]),
                             reads=["FC%d" % (h // 2), "spm"], writes=["FD%d" % (h // 2)])
                        P.op("act", lambda e, l=l, h=h: e.activation(out=FC[:, h, :], in_=FC[:, h, :], func=AF.Exp, scale=spm[:, l, 1, h:h + 1], bias=spm[:, l, 1, h:h + 1]),
                             reads=["FC%d" % (h // 2), "spm"], writes=["FC%d" % (h // 2)])
                        P.op("act", lambda e, l=l, h=h: e.activation(out=FA[:, h, :], in_=psh(8 + h), func=AF.Tanh, scale=0.5, bias=spm[:, l, 3, h:h + 1]),
                             reads=["ps%d" % (8 + h), "spm"], writes=["FA%d" % (h // 2)])
                    P.op("pool", lambda e: e.tensor_scalar(out=FC, in0=FC, scalar1=-1.0, scalar2=1.0, op0=ALU.mult, op1=ALU.add), reads=fres(2), writes=fres(2))
                    P.op("act", lambda e: e.activation(out=FC, in_=FC, func=AF.Sqrt), reads=fres(2), writes=fres(2))
                    P.op("dve", lambda e: e.scalar_tensor_tensor(out=FA, in0=FA, scalar=1.0, in1=FB, op0=ALU.add, op1=ALU.mult), reads=fres(0) + fres(1), writes=fres(0))
                    P.op("pool", lambda e: e.tensor_tensor(out=FC, in0=FC, in1=FA, op=ALU.mult), reads=fres(2) + fres(0), writes=fres(2))
                    for h in range(8):
                        P.op("dve", lambda e, l=l, h=h: e.tensor_tensor_scan(out=FA[:, h, :], data0=FD[:, h, :], data1=FC[:, h, :], initial=hst[:, l, h:h + 1], op0=ALU.mult, op1=ALU.add),
                             reads=["FD%d" % (h // 2), "FC%d" % (h // 2), "hst", "FA%d" % (h // 2)], writes=["FA%d" % (h // 2)])
                    P.op("pool", lambda e, l=l: e.tensor_copy(out=hst[:, l, :], in_=FA[:, :, T - 1]), reads=fres(0) + ["hst"], writes=["hst"])
                    fm_matmul(l, 3, hT, ["hT"], 0)
                    for b in range(4):
                        gelu2(psb(b), FB[:, 2 * b:2 * b + 2, :].rearrange("p m t -> p (m t)"), psn(2 * b, 2 * b + 2), ["FB%d" % b])
                        P.op("pool", lambda e, b=b: e.tensor_tensor(out=BB[:, 2 * b:2 * b + 2, :], in0=FB[:, 2 * b:2 * b + 2, :], in1=FA[:, 2 * b:2 * b + 2, :], op=ALU.mult),
                             reads=["FB%d" % b, "FA%d" % b], writes=["BB%d" % b])
                    fm_matmul(l, 4, hT, ["hT"], 8)
                    for b in range(4):
                        P.op("act", lambda e, b=b: e.activation(out=FC[:, 2 * b:2 * b + 2, :], in_=psb(4 + b).rearrange("p (m t) -> p m t", t=T), func=AF.Tanh, scale=0.5),
                             reads=psn(8 + 2 * b, 10 + 2 * b), writes=["FC%d" % b])
                    fm_matmul(l, 5, hT, ["hT"], 0)
                    for b in range(4):
                        P.op("act", lambda e, b=b: e.activation(out=FD[:, 2 * b:2 * b + 2, :], in_=psb(b).rearrange("p (m t) -> p m t", t=T), func=AF.Tanh, scale=0.5),
                             reads=psn(2 * b, 2 * b + 2), writes=["FD%d" % b])
                    fm_matmul(l, 6, BA, ["BA%d" % b for b in range(4)], 8)
                    for b in range(4):
                        P.op("dve", lambda e, b=b: e.scalar_tensor_tensor(out=FC[:, 2 * b:2 * b + 2, :], in0=FC[:, 2 * b:2 * b + 2, :], scalar=1.0,
                             in1=psb(4 + b).rearrange("p (m t) -> p m t", t=T), op0=ALU.add, op1=ALU.mult), reads=["FC%d" % b] + psn(8 + 2 * b, 10 + 2 * b), writes=["FC%d" % b])
                    fm_matmul(l, 7, BB, ["BB%d" % b for b in range(4)], 0)
                    for b in range(4):
                        P.op("dve", lambda e, b=b: e.scalar_tensor_tensor(out=FD[:, 2 * b:2 * b + 2, :], in0=FD[:, 2 * b:2 * b + 2, :], scalar=1.0,
                             in1=psb(b).rearrange("p (m t) -> p m t", t=T), op0=ALU.add, op1=ALU.mult), reads=["FD%d" % b] + psn(2 * b, 2 * b + 2), writes=["FD%d" % b])
                        P.op("dve", lambda e, b=b: e.scalar_tensor_tensor(out=BA[:, 2 * b:2 * b + 2, :], in0=FC[:, 2 * b:2 * b + 2, :], scalar=2.0,
                             in1=FD[:, 2 * b:2 * b + 2, :], op0=ALU.mult, op1=ALU.add), reads=["FC%d" % b, "FD%d" % b], writes=["BA%d" % b])
                    fm_matmul(l, 8, BA, ["BA%d" % b for b in range(4)], 8)
                    for m in range(8):
                        P.op("dve", lambda e, m=m, d=d: e.scalar_tensor_tensor(out=FC[:, m, :], in0=psh(8 + m), scalar=d(2)[:, m:m + 1], in1=xT[:, m, :], op0=ALU.mult, op1=ALU.add),
                             reads=["ps%d" % (8 + m), "dv", "xT"], writes=["FC%d" % (m // 2)])
                    layer_norm_fm(FC, fres(2), FD, fres(3), EPS / (ALPHA * ALPHA))
                    affine_fm(FC, fres(2), FD, fres(3), xT[:], "xT", pv("ln1_g", l), pv("ln1_b", l), ["prm"])
                    affine_fm(FC, fres(2), FA, fres(0), hT[:], "hT", d(3), d(4), ["dv"])
                    if l == 0 and it == 0 and s == 0:
                        dump("x1", xT[:], ["xT"])
                    for u in range(2):
                        for j in range(8):
                            P.op("pe", lambda e, u=u, j=j, l=l: e.matmul(psh(2)[:, u * NE:(u + 1) * NE], lhsT=hT[:, j, u * 128:(u + 1) * 128], rhs=wr[:, l, j, :], start=(j == 0), stop=(j == 7)),
                                 reads=["hT", "wr"], writes=["ps2"])
                    lg = psh(2)[:, 0:2 * NE].rearrange("p (u n) -> p u n", u=2)
                    P.op("act", lambda e: e.activation(out=r_sc[:], in_=lg, func=AF.Tanh, scale=0.5), reads=["ps2"], writes=["r_sc"])
                    P.op("dve", lambda e: e.tensor_scalar(out=r_sc[:], in0=r_sc[:], scalar1=0.5, scalar2=0.5, op0=ALU.mult, op1=ALU.add), reads=["r_sc"], writes=["r_sc"])
                    P.op("dve", lambda e, l=l: e.tensor_tensor(out=r_bi[:], in0=r_sc[:], in1=rb[:, l, :].unsqueeze(1).to_broadcast([128, 2, NE]), op=ALU.add), reads=["r_sc", "rb"], writes=["r_bi"])
                    bi4 = r_bi[:].rearrange("p u (g k) -> p u g k", k=8)
                    P.op("dve", lambda e: e.tensor_reduce(out=r_m1[:], in_=bi4, axis=AX.X, op=ALU.max), reads=["r_bi"], writes=["r_m1"])
                    t4 = r_t[:].rearrange("p u (g k) -> p u g k", k=8)
                    P.op("dve", lambda e: e.tensor_tensor(out=t4, in0=bi4, in1=r_m1[:].unsqueeze(3).to_broadcast([128, 2, 8, 8]), op=ALU.is_ge), reads=["r_bi", "r_m1"], writes=["r_t"])
                    P.op("dve", lambda e: e.scalar_tensor_tensor(out=r_t[:], in0=r_t[:], scalar=-1e9, in1=r_bi[:], op0=ALU.mult, op1=ALU.add), reads=["r_t", "r_bi"], writes=["r_t"])
                    P.op("dve", lambda e: e.tensor_reduce(out=r_m2[:], in_=t4, axis=AX.X, op=ALU.max), reads=["r_t"], writes=["r_m2"])
                    P.op("dve", lambda e: e.tensor_tensor(out=r_m1[:], in0=r_m1[:], in1=r_m2[:], op=ALU.add), reads=["r_m1", "r_m2"], writes=["r_m1"])
                    for u in range(2):
                        P.op("dve", lambda e, u=u: e.max(out=r_s8[:, u, :], in_=r_m1[:, u, :]), reads=["r_m1"], writes=["r_s8"])
                    for u in range(2):
                        P.op("dve", lambda e, u=u: e.tensor_scalar(out=r_k[:, u, :], in0=r_m1[:, u, :], scalar1=r_s8[:, u, 3:4], scalar2=None, op0=ALU.is_ge), reads=["r_m1", "r_s8"], writes=["r_k"])
                    kb = r_k[:].unsqueeze(3).to_broadcast([128, 2, 8, 8])
                    mb4 = r_mb[:].rearrange("p u (g k) -> p u g k", k=8)
                    P.op("dve", lambda e: e.tensor_tensor(out=mb4, in0=bi4, in1=kb, op=ALU.mult), reads=["r_bi", "r_k"], writes=["r_mb"])
                    P.op("dve", lambda e: e.tensor_scalar(out=r_k[:], in0=r_k[:], scalar1=1e9, scalar2=-1e9, op0=ALU.mult, op1=ALU.add), reads=["r_k", "r_mb"], writes=["r_k"])
                    P.op("dve", lambda e: e.tensor_tensor(out=mb4, in0=mb4, in1=kb, op=ALU.add), reads=["r_mb", "r_k"], writes=["r_mb"])
                    for u in range(2):
                        P.op("dve", lambda e, u=u: e.max(out=r_e8[:, u, :], in_=r_mb[:, u, :]), reads=["r_mb"], writes=["r_e8"])
                    for u in range(2):
                        P.op("dve", lambda e, u=u: e.tensor_scalar(out=r_w[:, u, :], in0=r_mb[:, u, :], scalar1=r_e8[:, u, 7:8], scalar2=None, op0=ALU.is_ge), reads=["r_mb", "r_e8"], writes=["r_w"])
                    P.op("dve", lambda e: e.tensor_tensor(out=r_w[:], in0=r_w[:], in1=r_sc[:], op=ALU.mult), reads=["r_w", "r_sc"], writes=["r_w"])
                    P.op("dve", lambda e: e.tensor_reduce(out=r_d[:], in_=r_w[:], axis=AX.X, op=ALU.add), reads=["r_w"], writes=["r_d"])
                    P.op("dve", lambda e: e.reciprocal(out=r_d[:], in_=r_d[:]), reads=["r_d"], writes=["r_d"])
                    for u in range(2):
                        P.op("dve", lambda e, u=u: e.tensor_scalar(out=r_G[:, u, :], in0=r_w[:, u, :], scalar1=r_d[:, u:u + 1], scalar2=1.25, op0=ALU.mult, op1=ALU.mult), reads=["r_w", "r_d"], writes=["r_G"])
                    slots = {}
                    NU = 2 * (NE + 1)

                    def up(n):
                        ex, u = n // 2, n % 2
                        if u == 0:
                            slots[ex] = wload(wexp_d[l][ex], 6144)
                        slot = slots[ex]
                        p = n % 2
                        w13 = ring[:, slot, 0:4096].rearrange("p (j n) -> p j n", n=512)
                        for j in range(8):
                            P.op("pe", lambda e, w13=w13, j=j, u=u, p=p: e.matmul(psb(p), lhsT=hT[:, j, u * 128:(u + 1) * 128], rhs=w13[:, j, :], start=(j == 0), stop=(j == 7)),
                                 reads=["ring%d" % slot, "hT"], writes=["pb%d" % p])
                        h1 = psb(p)[:, 0:256]
                        h3 = psb(p)[:, 256:512]
                        P.op("act", lambda e: e.activation(out=tbq[p][:], in_=h1, func=AF.Tanh, scale=0.5), reads=["pb%d" % p], writes=["tb%d" % p])
                        P.op("dve", lambda e: e.scalar_tensor_tensor(out=tbq[p][:], in0=tbq[p][:], scalar=1.0, in1=h1, op0=ALU.add, op1=ALU.mult), reads=["tb%d" % p, "pb%d" % p], writes=["tb%d" % p])
                        if ex < NE:
                            P.op("dve", lambda e: e.scalar_tensor_tensor(out=htok[p][:], in0=tbq[p][:], scalar=r_G[:, u, ex:ex + 1], in1=h3, op0=ALU.mult, op1=ALU.mult),
                                 reads=["tb%d" % p, "pb%d" % p, "r_G"], writes=["ht%d" % p])
                        else:
                            P.op("dve", lambda e: e.scalar_tensor_tensor(out=htok[p][:], in0=tbq[p][:], scalar=0.5, in1=h3, op0=ALU.mult, op1=ALU.mult),
                                 reads=["tb%d" % p, "pb%d" % p], writes=["ht%d" % p])

                    def tp(n):
                        p = n % 2
                        q = n % 3
                        for fc in range(2):
                            P.op("pe", lambda e, fc=fc: e.transpose(out=tpv[p][:, fc * 128:(fc + 1) * 128], in_=htok[p][:, fc * 128:(fc + 1) * 128], identity=identb[:]),
                                 reads=["ht%d" % p, "identb"], writes=["pb%d" % (2 + p)])
                        P.op("act", lambda e: e.copy(out=hx[q][:], in_=tpv[p][:, 0:256].rearrange("p (f t) -> p f t", f=2)), reads=["pb%d" % (2 + p)], writes=["hx%d" % q])

                    def down(n):
                        ex, u = n // 2, n % 2
                        q = n % 3
                        slot = slots[ex]
                        w2 = ring[:, slot, 4096:6144].rearrange("p (f o) -> p f o", f=2)
                        for half in range(2):
                            for fc in range(2):
                                P.op("pe", lambda e, w2=w2, half=half, fc=fc: e.matmul(psb(4 + u * 2 + half), lhsT=hx[q][:, fc, :], rhs=w2[:, fc, half * 512:(half + 1) * 512],
                                     start=(ex == 0 and fc == 0), stop=(ex == NE and fc == 1)),
                                     reads=["ring%d" % slot, "hx%d" % q], writes=["pb%d" % (4 + u * 2 + half)])
                    for n in range(NU + 2):
                        if n < NU:
                            up(n)
                        if 1 <= n <= NU:
                            tp(n - 1)
                        if n >= 2:
                            down(n - 2)
                    for u in range(2):
                        for half in range(2):
                            b = u * 2 + half
                            if half == 0:
                                P.op("act", lambda e, u=u, half=half, b=b: e.copy(out=xtok[:, u, half * 512:(half + 1) * 512], in_=psb(4 + b)), reads=["pb%d" % (4 + b)], writes=["vg%d" % b])
                            else:
                                P.op("dve", lambda e, u=u, half=half, b=b: e.tensor_copy(out=xtok[:, u, half * 512:(half + 1) * 512], in_=psb(4 + b)), reads=["pb%d" % (4 + b)], writes=["vg%d" % b])
                    for m in range(8):
                        for u in range(2):
                            P.op("pe", lambda e, m=m, u=u: e.transpose(out=psh(m)[:, u * 128:(u + 1) * 128], in_=xtok[:, u, m * 128:(m + 1) * 128], identity=ident[:]),
                                 reads=["vg%d" % (u * 2 + m // 4), "ident"], writes=["ps%d" % m])
                    for m in range(8):
                        P.op("dve", lambda e, m=m, d=d: e.scalar_tensor_tensor(out=FC[:, m, :], in0=psh(m), scalar=d(5)[:, m:m + 1], in1=xT[:, m, :], op0=ALU.mult, op1=ALU.add),
                             reads=["ps%d" % m, "dv", "xT"], writes=["FC%d" % (m // 2)])
                    layer_norm_fm(FC, fres(2), FD, fres(3), EPS / (ALPHA * ALPHA))
                    affine_fm(FC, fres(2), FD, fres(3), xT[:], "xT", pv("ln2_g", l), pv("ln2_b", l), ["prm"])
                    if l == 0 and it == 0 and s == 0:
                        dump("x2", xT[:], ["xT"])
                for u in range(2):
                    for m in range(8):
                        P.op("pe", lambda e, u=u, m=m: e.transpose(out=psb(u * 2 + m // 4)[:, (m % 4) * 128:(m % 4 + 1) * 128], in_=xT[:, m, u * 128:(u + 1) * 128], identity=ident[:]),
                             reads=["xT", "ident"], writes=psn(2 * (u * 2 + m // 4), 2 * (u * 2 + m // 4) + 2))
                    for half in range(2):
                        b = u * 2 + half
                        if half == 0:
                            P.op("act", lambda e, u=u, half=half, b=b: e.copy(out=xtok[:, u, half * 512:(half + 1) * 512], in_=psb(b)), reads=psn(2 * b, 2 * b + 2) + ["vg%d" % b], writes=["vg%d" % b])
                        else:
                            P.op("dve", lambda e, u=u, half=half, b=b: e.tensor_copy(out=xtok[:, u, half * 512:(half + 1) * 512], in_=psb(b)), reads=psn(2 * b, 2 * b + 2) + ["vg%d" % b], writes=["vg%d" % b])
                P.op("sp", lambda e, s=s, t0=t0: e.dma_start(out=out_d[s, t0:t0 + T, :].rearrange("(u p) d -> p u d", p=128), in_=xtok[:]),
                     reads=["vg%d" % b for b in range(4)], dma_sem="st")

        P.finalize()
        with nc.Block() as block:
            @block.sync
            def _(e):
                P.emit("sp", e, sems)
                for s_, v_ in P.dma_sem_count.items():
                    if s_ in ("st", "dbg"):
                        e.wait_ge(sems[s_], v_)

            @block.scalar
            def _(e):
                P.emit("act", e, sems)

            @block.vector
            def _(e):
                P.emit("dve", e, sems)

            @block.gpsimd
            def _(e):
                P.emit("pool", e, sems)

            @block.tensor
            def _(e):
                P.emit("pe", e, sems)
    return nc, P


def kernel(**inputs):
    ncores = 8
    x = np.ascontiguousarray(inputs["x"], dtype=np.float32)
    c = np.ascontiguousarray(inputs["c"], dtype=np.float32)
    nc, _ = build_program(nseq=1, nt=16, depth=4)
    wmap = {n: np.ascontiguousarray(inputs[n], dtype=np.float32) for n in WNAMES}
    out = np.empty_like(x)
    for k in range(2):
        in_maps = []
        for i in range(ncores):
            m = dict(wmap)
            m["x"] = x[2 * i + k:2 * i + k + 1]
            m["c"] = c[2 * i + k:2 * i + k + 1]
            in_maps.append(m)
        res = run_bass_kernel_spmd(nc, in_maps, core_ids=list(range(ncores)))
        for i in range(ncores):
            out[2 * i + k] = res.results[i]["out"][0]
    return out
```

```python
import contextlib
import numpy as np
import concourse.bass as bass
import concourse.mybir as mybir
from concourse.bass_utils import run_bass_kernel_spmd

F32 = mybir.dt.float32
BF16 = mybir.dt.bfloat16
AF = mybir.ActivationFunctionType
ALU = mybir.AluOpType
AX = mybir.AxisListType

L_FULL = 4
D = 1024
T = 256
NE = 64
ALPHA = 8.0 ** 0.25
EPS = 1e-5
C_G = 0.7978845608028654
SQ_G = 0.044715 ** 0.5
NSLOT = 4


class Prog:
    ENGS = ("pe", "act", "dve", "pool", "sp")

    def __init__(self):
        self.ops = {e: [] for e in self.ENGS}
        self.res = {}
        self.dma_sem_count = {}

    @staticmethod
    def _canon(names):
        out = []
        for n in names:
            if n.startswith("ps") and n[2:].isdigit():
                n = "pb%d" % (int(n[2:]) // 2)
            if n not in out:
                out.append(n)
        return out

    def op(self, eng, fn, reads=(), writes=(), dma_sem=None, after=()):
        reads = self._canon(reads)
        writes = self._canon(writes)
        idx = len(self.ops[eng])
        me = (eng, idx)
        deps = set(after)
        for r in reads:
            st = self.res.get(r)
            if st is None:
                st = self.res[r] = {"w": None, "r": {}, "rd": []}
            if st["w"] is not None:
                deps.add(st["w"])
        for w in writes:
            st = self.res.get(w)
            if st is None:
                st = self.res[w] = {"w": None, "r": {}, "rd": []}
            if st["w"] is not None:
                deps.add(st["w"])
            for e2, i2 in st["r"].items():
                deps.add((e2, i2))
            for d in st["rd"]:
                deps.add(d)
        if eng == "pe":
            deps = {d for d in deps if d[0] != "pe"}
        deps.discard(me)
        rec = {"fn": fn, "deps": deps, "signal": False, "dma_sem": dma_sem, "tok": None}
        self.ops[eng].append(rec)
        is_dma = dma_sem is not None
        for r in reads:
            st = self.res[r]
            if is_dma:
                st["rd"].append(me)
            else:
                st["r"][eng] = idx
        for w in writes:
            self.res[w] = {"w": me, "r": {}, "rd": []}
        return me

    def finalize(self):
        for e in self.ENGS:
            for rec in self.ops[e]:
                for (de, di) in rec["deps"]:
                    self.ops[de][di]["signal"] = True
        for e in self.ENGS:
            cnt = 0
            for rec in self.ops[e]:
                if rec["dma_sem"] is not None:
                    s = rec["dma_sem"]
                    self.dma_sem_count[s] = self.dma_sem_count.get(s, 0) + 16
                    rec["tok"] = (s, self.dma_sem_count[s])
                    rec["signal"] = True
                elif rec["signal"]:
                    cnt += 1
                    rec["tok"] = (e, cnt)

    def emit(self, eng_name, eng, sems):
        waited = {}
        for rec in self.ops[eng_name]:
            need = {}
            for (de, di) in rec["deps"]:
                s, v = self.ops[de][di]["tok"]
                if need.get(s, 0) < v:
                    need[s] = v
            todo = []
            for s, v in need.items():
                if waited.get(s, 0) >= v:
                    continue
                todo.append((s, v))
                waited[s] = v
            fold = None
            if todo and eng_name != "sp":
                fold = todo.pop()
            for s, v in todo:
                eng.wait_ge(sems[s], v)
            inst = rec["fn"](eng)
            if fold is not None:
                inst._wait_ge(sems[fold[0]], fold[1])
            if rec["signal"]:
                s, v = rec["tok"]
                inst.then_inc(sems[s], 16 if rec["dma_sem"] is not None else 1)


WNAMES = ["ada_w", "ada_b", "w_in", "sgu_ln_g", "sgu_ln_b", "sgu_w", "sgu_b", "conv_w", "conv_b",
          "rg_wa", "rg_ba", "rg_wx", "rg_bx", "rg_lambda", "w_branch_a", "w_branch_b", "w_out",
          "ln1_g", "ln1_b", "router_w", "router_b", "exp_w1", "exp_w3", "exp_w2", "sh_w1", "sh_w3",
          "sh_w2", "ln2_g", "ln2_b"]
WSHAPES = {
    "ada_w": [4, 1024, 6144], "ada_b": [4, 6144], "w_in": [4, 1024, 6144], "sgu_ln_g": [4, 1024],
    "sgu_ln_b": [4, 1024], "sgu_w": [4, 8, 128, 128], "sgu_b": [4, 8, 128], "conv_w": [4, 4, 1024],
    "conv_b": [4, 1024], "rg_wa": [4, 8, 128, 128], "rg_ba": [4, 1024], "rg_wx": [4, 8, 128, 128],
    "rg_bx": [4, 1024], "rg_lambda": [4, 1024], "w_branch_a": [4, 1024, 1024],
    "w_branch_b": [4, 1024, 1024], "w_out": [4, 1024, 1024], "ln1_g": [4, 1024], "ln1_b": [4, 1024],
    "router_w": [4, 1024, 64], "router_b": [4, 64], "exp_w1": [4, 64, 1024, 256],
    "exp_w3": [4, 64, 1024, 256], "exp_w2": [4, 64, 256, 1024], "sh_w1": [4, 1024, 256],
    "sh_w3": [4, 1024, 256], "sh_w2": [4, 256, 1024], "ln2_g": [4, 1024], "ln2_b": [4, 1024],
}
PV = ["sgu_ln_g", "sgu_ln_b", "conv_b", "rg_ba", "rg_bx", "rg_lambda", "ln1_g", "ln1_b", "ln2_g", "ln2_b",
      "cw0", "cw1", "cw2", "cw3", "ab0", "ab1", "ab2", "ab3", "ab4", "ab5"]


def build_program(nseq=2, nt=16, depth=4, seq=4096, dbg=None, phases="ABC", nlayers_cast=None):
    nc = bass.Bass("TRN2", target_bir_lowering=False)
    P = Prog()
    LD = depth
    x_d = nc.dram_tensor("x", [nseq, seq, D], F32, kind="ExternalInput").ap()
    c_d = nc.dram_tensor("c", [nseq, D], F32, kind="ExternalInput").ap()
    W = {n: nc.dram_tensor(n, WSHAPES[n], F32, kind="ExternalInput").ap() for n in WNAMES}
    out_d = nc.dram_tensor("out", [nseq, nt * T, D], F32, kind="ExternalOutput").ap()
    wmix_d = [nc.dram_tensor("wmix%d" % l, [18, 128, 4096], BF16, kind="Internal").ap() for l in range(LD)]
    wexp_d = [nc.dram_tensor("wexp%d" % l, [NE + 1, 128, 6144], BF16, kind="Internal").ap() for l in range(LD)]
    dbg_d = {}
    if dbg:
        for name, shape in dbg.items():
            dbg_d[name] = nc.dram_tensor("dbg_" + name, shape, F32, kind="ExternalOutput").ap()

    es = contextlib.ExitStack()
    with es:
        def sb(name, shape, dt=F32):
            return es.enter_context(nc.sbuf_tensor(name, shape, dt))

        ident = sb("ident", [128, 128]); onesD = sb("onesD", [128, 128]); onesb = sb("onesb", [128, 128], BF16)
        identb = sb("identb", [128, 128], BF16); tril = sb("tril", [128, 128])
        stgp = sb("stgp", [128, 6, 128]); prm = sb("prm", [128, 6, 128])
        adaS = sb("adaS", [128, LD, 6, 8, nseq]); dv = sb("dv", [128, LD, nseq, 6, 8])
        cact = sb("cact", [128, 16])
        spm = sb("spm", [128, LD, 4, 8])
        WsT = sb("WsT", [128, LD, 8, 128], BF16); bias2 = sb("bias2", [128, LD, 8, 128])
        rgw = sb("rgw", [128, LD, 2, 8, 128], BF16); wr = sb("wr", [128, LD, 8, NE], BF16)
        rb = sb("rb", [128, LD, NE])
        ring = sb("ring", [128, NSLOT, 6144], BF16)
        xT = sb("xT", [128, 8, T]); hT = sb("hT", [128, 8, T], BF16)
        xtok = sb("xtok", [128, 2, D]); scr = sb("scr", [128, 8192])
        BA = sb("BA", [128, 8, T], BF16); BB = sb("BB", [128, 8, T], BF16)
        rin = sb("rin", [128, 8, T + 3])
        mS = sb("mS", [128, T]); vS = sb("vS", [128, T]); rS = sb("rS", [128, T])
        st6 = sb("st6", [128, 2, 2, 6]); mv = sb("mv", [128, 2, 2]); rstd = sb("rstd", [128, 2])
        r_sc = sb("r_sc", [128, 2, NE]); r_bi = sb("r_bi", [128, 2, NE]); r_t = sb("r_t", [128, 2, NE])
        r_m1 = sb("r_m1", [128, 2, 8]); r_m2 = sb("r_m2", [128, 2, 8]); r_k = sb("r_k", [128, 2, 8])
        r_s8 = sb("r_s8", [128, 2, 8]); r_mb = sb("r_mb", [128, 2, NE]); r_e8 = sb("r_e8", [128, 2, 8])
        r_w = sb("r_w", [128, 2, NE]); r_d = sb("r_d", [128, 2]); r_G = sb("r_G", [128, 2, NE])
        GT = sb("GT", [128, T], BF16)
        tbq = [sb("tb0", [128, 256]), sb("tb1", [128, 256])]
        htok = [sb("ht0", [128, 256], BF16), sb("ht1", [128, 256], BF16)]
        hx = [sb("hx%d" % i, [128, 2, 128], BF16) for i in range(3)]
        convh = sb("convh", [128, LD, 8, 3]); hst = sb("hst", [128, LD, 8])
        ps = es.enter_context(nc.psum_tensor("ps", [128, 4096], F32))

        sem_names = list(Prog.ENGS) + ["wl%d" % k for k in range(NSLOT)] + ["ws%d" % k for k in range(NSLOT)] + \
            ["sg%d" % k for k in range(4)] + ["pl", "xl", "st", "dbg"]
        sems = {s: es.enter_context(nc.semaphore(s)) for s in sem_names}

        Fv = [scr[:, i * 2048:(i + 1) * 2048].rearrange("p (m t) -> p m t", t=T) for i in range(4)]
        Fn = ["FA", "FB", "FC", "FD"]
        gB = scr[:].bitcast(BF16).rearrange("p (e t) -> p e t", t=T)
        vn = BB[:].rearrange("p m t -> p (m t)").rearrange("p (s c) -> p s c", s=2)
        STG = [scr[:, i * 2048:(i + 1) * 2048] for i in range(4)]

        tpv = [ps[:, (2 + k) * 512:(3 + k) * 512].bitcast(BF16) for k in range(2)]

        def psh(i):
            return ps[:, i * 256:(i + 1) * 256]

        def psb(b):
            return ps[:, b * 512:(b + 1) * 512]

        def psn(lo, hi):
            return ["ps%d" % i for i in range(lo, hi)]

        def fres(k, banks=range(4)):
            return ["%s%d" % (Fn[k], b) for b in banks]

        def prmv(v, l):
            return prm[:, v // 4, (v % 4) * 32 + l * 8:(v % 4) * 32 + l * 8 + 8]

        def pv(name, l):
            return prmv(PV.index(name), l)

        def bc_t(ap8):
            return ap8.unsqueeze(2).to_broadcast([128, 8, T])

        P.op("pool", lambda e: e.memset(ident[:], 1.0), writes=["ident"])
        P.op("pool", lambda e: e.affine_select(out=ident[:], in_=ident[:], pattern=[[-1, 128]], compare_op=ALU.is_equal, fill=0.0, base=0, channel_multiplier=1), reads=["ident"], writes=["ident"])
        P.op("pool", lambda e: e.memset(tril[:], 1.0), writes=["tril"])
        P.op("pool", lambda e: e.affine_select(out=tril[:], in_=tril[:], pattern=[[-1, 128]], compare_op=ALU.is_ge, fill=0.0, base=0, channel_multiplier=1), reads=["tril"], writes=["tril"])
        P.op("pool", lambda e: e.memset(onesD[:], 1.0 / D), writes=["onesD"])
        P.op("pool", lambda e: e.memset(onesb[:], 1.0), writes=["onesb"])
        P.op("pool", lambda e: e.tensor_copy(out=identb[:], in_=ident[:]), reads=["ident"], writes=["identb"])
        P.op("pool", lambda e: e.memset(stgp[:], 0.0), writes=["stgp"])

        pl_ops = []
        ms = P.ops["pool"]
        stgp_init = ("pool", len(ms) - 1)

        def pload(v, l, src):
            b, o = v // 4, (v % 4) * 32 + l * 8
            pl_ops.append(P.op("sp", lambda e: e.dma_start(out=stgp[o:o + 8, b, :], in_=src), dma_sem="pl", after=[stgp_init]))
        for l in range(LD):
            for v, name in enumerate(PV):
                if name.startswith("cw"):
                    src = W["conv_w"][l, int(name[2]), :].rearrange("(j p) -> j p", p=128)
                elif name.startswith("ab"):
                    k = int(name[2])
                    src = W["ada_b"][l, k * D:(k + 1) * D].rearrange("(j p) -> j p", p=128)
                else:
                    src = W[name][l, :].rearrange("(j p) -> j p", p=128)
                pload(v, l, src)
        pl_ops.append(P.op("sp", lambda e: e.dma_start(out=stgp[0:nseq * 8, 5, :], in_=c_d.rearrange("s (j p) -> (s j) p", p=128)), dma_sem="pl", after=[stgp_init]))
        for b in range(6):
            P.op("pe", lambda e, b=b: e.transpose(out=psh(b)[:, 0:128], in_=stgp[:, b, :], identity=ident[:]), reads=["ident"], writes=["ps%d" % b], after=pl_ops)
            P.op("dve", lambda e, b=b: e.tensor_copy(out=prm[:, b, :], in_=psh(b)[:, 0:128]), reads=["ps%d" % b], writes=["prm"])
        ncol = nseq * 8
        P.op("act", lambda e: e.activation(out=cact[:, 0:ncol], in_=prm[:, 5, 0:ncol], func=AF.Tanh, scale=0.5), reads=["prm"], writes=["cact"])
        P.op("dve", lambda e: e.scalar_tensor_tensor(out=cact[:, 0:ncol], in0=cact[:, 0:ncol], scalar=1.0, in1=prm[:, 5, 0:ncol], op0=ALU.add, op1=ALU.mult), reads=["cact", "prm"], writes=["cact"])
        P.op("dve", lambda e: e.tensor_scalar(out=cact[:, 0:ncol], in0=cact[:, 0:ncol], scalar1=0.5, scalar2=None, op0=ALU.mult), reads=["cact"], writes=["cact"])
        for l in range(LD):
            P.op("act", lambda e, l=l: e.activation(out=spm[:, l, 0, :], in_=pv("rg_lambda", l), func=AF.Exp, scale=-1.0), reads=["prm"], writes=["spm"])
        for l in range(LD):
            P.op("act", lambda e, l=l: e.activation(out=spm[:, l, 0, :], in_=spm[:, l, 0, :], func=AF.Ln, bias=1.0), reads=["spm"], writes=["spm"])
        for l in range(LD):
            P.op("dve", lambda e, l=l: e.tensor_scalar(out=spm[:, l, 1, :], in0=spm[:, l, 0, :], scalar1=-8.0, scalar2=None, op0=ALU.mult), reads=["spm"], writes=["spm"])
            P.op("dve", lambda e, l=l: e.tensor_scalar(out=spm[:, l, 0, :], in0=spm[:, l, 0, :], scalar1=-4.0, scalar2=None, op0=ALU.mult), reads=["spm"], writes=["spm"])
            P.op("dve", lambda e, l=l: e.tensor_scalar(out=spm[:, l, 2, :], in0=pv("rg_ba", l), scalar1=0.5, scalar2=None, op0=ALU.mult), reads=["prm"], writes=["spm"])
            P.op("dve", lambda e, l=l: e.tensor_scalar(out=spm[:, l, 3, :], in0=pv("rg_bx", l), scalar1=0.5, scalar2=None, op0=ALU.mult), reads=["prm"], writes=["spm"])

        adaP = ps[:, 3072:3072 + LD * 48 * nseq].rearrange("p (l v m s) -> p l v m s", l=LD, v=6, m=8)
        adaPn = psn(12, 16)
        sgi = 0
        import os as _os
        _pha = _os.environ.get('PHA', '123')
        for l in (range(LD) if '2' in _pha else []):
            for q in range(24):
                k = sgi % 4
                sgi += 1
                stg = STG[k].rearrange("p (j n) -> p j n", n=256)
                src = W["ada_w"][l, :, q * 256:(q + 1) * 256].rearrange("(j p) n -> p j n", p=128)
                P.op("sp", lambda e, stg=stg, src=src: e.dma_start(out=stg, in_=src), writes=["stg%d" % k], dma_sem="sg%d" % k)
                for mm in range(2):
                    v, m = (q * 2 + mm) // 8, (q * 2 + mm) % 8
                    for j in range(8):
                        P.op("pe", lambda e, stg=stg, l=l, v=v, m=m, j=j, mm=mm: e.matmul(
                            adaP[:, l, v, m, :], lhsT=stg[:, j, mm * 128:(mm + 1) * 128],
                            rhs=(cact[:, j:j + 9:8] if nseq == 2 else cact[:, j:j + 1]), start=(j == 0), stop=(j == 7)),
                            reads=["stg%d" % k, "cact"], writes=adaPn)
        for l in (range(LD) if '2' in _pha else []):
            for k6 in range(6):
                vv = 14 + k6
                P.op("dve", lambda e, l=l, k6=k6, vv=vv: e.tensor_tensor(
                    out=adaS[:, l, k6, :, :], in0=adaP[:, l, k6, :, :],
                    in1=prmv(vv, l).unsqueeze(2).to_broadcast([128, 8, nseq]), op=ALU.add),
                    reads=adaPn + ["prm"], writes=["adaS"])
        for l in (range(LD) if '2' in _pha else []):
            for s in range(nseq):
                A = lambda k6, l=l, s=s: adaS[:, l, k6, :, s]
                o = lambda k, l=l, s=s: dv[:, l, s, k, :]
                P.op("dve", lambda e, A=A, o=o: e.tensor_scalar(out=o(0), in0=A(1), scalar1=1.0, scalar2=None, op0=ALU.add), reads=["adaS"], writes=["dv"])
                P.op("dve", lambda e, A=A, o=o: e.tensor_copy(out=o(1), in_=A(0)), reads=["adaS"], writes=["dv"])
                P.op("dve", lambda e, A=A, o=o: e.tensor_scalar(out=o(2), in0=A(2), scalar1=1.0 / (8.0 * ALPHA), scalar2=None, op0=ALU.mult), reads=["adaS"], writes=["dv"])
                P.op("dve", lambda e, A=A, o=o: e.tensor_scalar(out=o(5), in0=A(4), scalar1=1.0, scalar2=None, op0=ALU.add), reads=["adaS"], writes=["dv"])
                P.op("dve", lambda e, A=A, o=o, l=l: e.tensor_tensor(out=o(3), in0=o(5), in1=pv("ln1_g", l), op=ALU.mult), reads=["dv", "prm"], writes=["dv"])
                P.op("dve", lambda e, A=A, o=o, l=l: e.tensor_tensor(out=o(4), in0=o(5), in1=pv("ln1_b", l), op=ALU.mult), reads=["dv", "prm"], writes=["dv"])
                P.op("dve", lambda e, A=A, o=o: e.tensor_tensor(out=o(4), in0=o(4), in1=A(3), op=ALU.add), reads=["dv", "adaS"], writes=["dv"])
                P.op("dve", lambda e, A=A, o=o: e.tensor_scalar(out=o(5), in0=A(5), scalar1=1.0 / ALPHA, scalar2=None, op0=ALU.mult), reads=["adaS", "dv"], writes=["dv"])

        for l in (range(LD) if '3' in _pha else []):
            k = sgi % 4
            sgi += 1
            stg = STG[k][:, 0:1024].rearrange("p (g s) -> p g s", s=128)
            P.op("sp", lambda e, stg=stg, l=l: e.dma_start(out=stg, in_=W["sgu_w"][l].rearrange("g t s -> t g s")), writes=["stg%d" % k], dma_sem="sg%d" % k)
            P.op("pool", lambda e, stg=stg: e.tensor_tensor(out=stg, in0=stg, in1=tril[:].unsqueeze(1).to_broadcast([128, 8, 128]), op=ALU.mult), reads=["stg%d" % k, "tril"], writes=["stg%d" % k])
            for g in range(8):
                P.op("pe", lambda e, stg=stg, g=g: e.transpose(out=psh(g)[:, 0:128], in_=stg[:, g, :], identity=ident[:]), reads=["stg%d" % k, "ident"], writes=["ps%d" % g])
                P.op("act", lambda e, l=l, g=g: e.copy(out=WsT[:, l, g, :], in_=psh(g)[:, 0:128]), reads=["ps%d" % g], writes=["WsT"])
            k2 = sgi % 4
            sgi += 1
            stg2 = STG[k2][:, 0:1024]
            P.op("sp", lambda e, stg2=stg2, l=l: e.dma_start(out=stg2, in_=W["sgu_b"][l:l + 1].rearrange("o g t -> o (g t)").to_broadcast([128, 1024])), writes=["stg%d" % k2], dma_sem="sg%d" % k2)
            for g in range(8):
                P.op("pe", lambda e, l=l, g=g: e.matmul(psh(8 + g)[:, 0:128], lhsT=onesb[:], rhs=WsT[:, l, g, :], start=True, stop=True), reads=["onesb", "WsT"], writes=["ps%d" % (8 + g)])
                P.op("dve", lambda e, l=l, g=g, stg2=stg2: e.scalar_tensor_tensor(
                    out=bias2[:, l, g, :], in0=psh(8 + g)[:, 0:128], scalar=pv("sgu_ln_b", l)[:, g:g + 1],
                    in1=stg2[:, g * 128:(g + 1) * 128], op0=ALU.mult, op1=ALU.add),
                    reads=["ps%d" % (8 + g), "prm", "stg%d" % k2], writes=["bias2"])
            for wi, wn in enumerate(["rg_wa", "rg_wx"]):
                k3 = sgi % 4
                sgi += 1
                stg3 = STG[k3][:, 0:1024].rearrange("p (h j) -> p h j", j=128)
                P.op("sp", lambda e, stg3=stg3, l=l, wn=wn: e.dma_start(out=stg3, in_=W[wn][l].rearrange("h i j -> i h j")), writes=["stg%d" % k3], dma_sem="sg%d" % k3)
                P.op("dve", lambda e, stg3=stg3, l=l, wi=wi: e.tensor_copy(out=rgw[:, l, wi, :, :], in_=stg3), reads=["stg%d" % k3], writes=["rgw"])
            k4 = sgi % 4
            sgi += 1
            stg4 = STG[k4][:, 0:512].rearrange("p (j n) -> p j n", n=NE)
            P.op("sp", lambda e, stg4=stg4, l=l: e.dma_start(out=stg4, in_=W["router_w"][l].rearrange("(j p) n -> p j n", p=128)), writes=["stg%d" % k4], dma_sem="sg%d" % k4)
            P.op("dve", lambda e, stg4=stg4, l=l: e.tensor_copy(out=wr[:, l, :, :], in_=stg4), reads=["stg%d" % k4], writes=["wr"])
            P.op("sp", lambda e, l=l: e.dma_start(out=rb[:, l, :], in_=W["router_b"][l:l + 1, :].to_broadcast([128, NE])), writes=["rb"], dma_sem="pl")

        wstores = []
        first_wload = [True]
        MIXW = [("w_in", 1), ("w_in", 0), ("w_in", 3), ("w_in", 2), ("w_in", 4), ("w_in", 5),
                ("w_branch_a", None), ("w_branch_b", None), ("w_out", None)]
        cast_i = [0]
        slot_i = [0]

        sgi_box = [sgi]
        for l in (range(LD) if "B" in phases else []):
            for ci, (wn, slot_w) in enumerate(MIXW):
                for half in range(2):
                    slot = slot_i[0] % NSLOT
                    slot_i[0] += 1
                    for q in range(2):
                        c0 = half * 512 + q * 256
                        if wn == "w_in":
                            src = W[wn][l, :, slot_w * 1024 + c0: slot_w * 1024 + c0 + 256]
                        else:
                            src = W[wn][l, :, c0:c0 + 256]
                        src = src.rearrange("(j p) n -> p j n", p=128)
                        k = sgi_box[0] % 4
                        sgi_box[0] += 1
                        stgv = STG[k].rearrange("p (j n) -> p j n", n=256)
                        P.op("sp", lambda e, stgv=stgv, src=src: e.dma_start(out=stgv, in_=src), writes=["stg%d" % k], dma_sem="sg%d" % k)
                        dst = ring[:, slot, 0:4096].rearrange("p (j n) -> p j n", n=512)[:, :, q * 256:(q + 1) * 256]
                        eng = "act" if cast_i[0] % 2 == 0 else "dve"
                        cast_i[0] += 1
                        if eng == "act":
                            P.op("act", lambda e, dst=dst, stgv=stgv: e.copy(out=dst, in_=stgv), reads=["stg%d" % k], writes=["ring%d" % slot])
                        else:
                            P.op("dve", lambda e, dst=dst, stgv=stgv: e.tensor_copy(out=dst, in_=stgv), reads=["stg%d" % k], writes=["ring%d" % slot])
                    wstores.append(P.op("sp", lambda e, slot=slot, l=l, ci=ci, half=half: e.dma_start(out=wmix_d[l][ci * 2 + half], in_=ring[:, slot, 0:4096]),
                         reads=["ring%d" % slot], dma_sem="ws%d" % slot))
            for ex in range(NE + 1):
                slot = slot_i[0] % NSLOT
                slot_i[0] += 1
                if ex < NE:
                    s1, s3, s2 = W["exp_w1"][l, ex], W["exp_w3"][l, ex], W["exp_w2"][l, ex]
                else:
                    s1, s3, s2 = W["sh_w1"][l], W["sh_w3"][l], W["sh_w2"][l]
                for q, src in enumerate([s1, s3]):
                    src = src.rearrange("(j p) n -> p j n", p=128)
                    k = sgi_box[0] % 4
                    sgi_box[0] += 1
                    stgv = STG[k].rearrange("p (j n) -> p j n", n=256)
                    P.op("sp", lambda e, stgv=stgv, src=src: e.dma_start(out=stgv, in_=src), writes=["stg%d" % k], dma_sem="sg%d" % k)
                    dst = ring[:, slot, 0:4096].rearrange("p (j n) -> p j n", n=512)[:, :, q * 256:(q + 1) * 256]
                    eng = "act" if cast_i[0] % 2 == 0 else "dve"
                    cast_i[0] += 1
                    if eng == "act":
                        P.op("act", lambda e, dst=dst, stgv=stgv: e.copy(out=dst, in_=stgv), reads=["stg%d" % k], writes=["ring%d" % slot])
                    else:
                        P.op("dve", lambda e, dst=dst, stgv=stgv: e.tensor_copy(out=dst, in_=stgv), reads=["stg%d" % k], writes=["ring%d" % slot])
                src = s2.rearrange("(f p) o -> p f o", p=128)
                k = sgi_box[0] % 4
                sgi_box[0] += 1
                stgv = STG[k].rearrange("p (f o) -> p f o", f=2)
                P.op("sp", lambda e, stgv=stgv, src=src: e.dma_start(out=stgv, in_=src), writes=["stg%d" % k], dma_sem="sg%d" % k)
                dst = ring[:, slot, 4096:6144].rearrange("p (f o) -> p f o", f=2)
                eng = "act" if cast_i[0] % 2 == 0 else "dve"
                cast_i[0] += 1
                if eng == "act":
                    P.op("act", lambda e, dst=dst, stgv=stgv: e.copy(out=dst, in_=stgv), reads=["stg%d" % k], writes=["ring%d" % slot])
                else:
                    P.op("dve", lambda e, dst=dst, stgv=stgv: e.tensor_copy(out=dst, in_=stgv), reads=["stg%d" % k], writes=["ring%d" % slot])
                wstores.append(P.op("sp", lambda e, slot=slot, l=l, ex=ex: e.dma_start(out=wexp_d[l][ex], in_=ring[:, slot, :]),
                     reads=["ring%d" % slot], dma_sem="ws%d" % slot))

        def wload(src, width):
            slot = slot_i[0] % NSLOT
            slot_i[0] += 1
            aft = wstores if first_wload[0] else ()
            first_wload[0] = False
            P.op("sp", lambda e: e.dma_start(out=ring[:, slot, 0:width], in_=src),
                 writes=["ring%d" % slot], dma_sem="wl%d" % slot, after=aft)
            return slot

        def gelu2(src, dst, rsrc, rdst):
            P.op("act", lambda e: e.activation(out=dst, in_=src, func=AF.Square, scale=SQ_G), reads=rsrc, writes=rdst)
            P.op("dve", lambda e: e.scalar_tensor_tensor(out=dst, in0=dst, scalar=1.0, in1=src, op0=ALU.add, op1=ALU.mult), reads=rsrc + rdst, writes=rdst)
            P.op("act", lambda e: e.activation(out=dst, in_=dst, func=AF.Tanh, scale=C_G), reads=rdst, writes=rdst)
            P.op("dve", lambda e: e.scalar_tensor_tensor(out=dst, in0=dst, scalar=1.0, in1=src, op0=ALU.add, op1=ALU.mult), reads=rsrc + rdst, writes=rdst)

        def fm_matmul(l, ci, rhs_ap, rhs_res, ps_base):
            for half in range(2):
                slot = wload(wmix_d[l][ci * 2 + half], 4096)
                wv = ring[:, slot, 0:4096].rearrange("p (j n) -> p j n", n=512)
                for mm in range(4):
                    m = half * 4 + mm
                    for j in range(8):
                        P.op("pe", lambda e, wv=wv, mm=mm, m=m, j=j: e.matmul(
                            psh(ps_base + m), lhsT=wv[:, j, mm * 128:(mm + 1) * 128], rhs=rhs_ap[:, j, :],
                            start=(j == 0), stop=(j == 7)),
                            reads=["ring%d" % slot] + rhs_res, writes=["ps%d" % (ps_base + m)])

        def layer_norm_fm(res, res_names, sq, sq_names, eps):
            for b in range(4):
                P.op("act", lambda e, b=b: e.activation(out=sq[:, 2 * b:2 * b + 2, :], in_=res[:, 2 * b:2 * b + 2, :], func=AF.Square),
                     reads=[res_names[b]], writes=[sq_names[b]])
            for j in range(8):
                P.op("pe", lambda e, j=j: e.matmul(psh(0), lhsT=onesD[:], rhs=res[:, j, :], start=(j == 0), stop=(j == 7)),
                     reads=["onesD", res_names[j // 2]], writes=["ps0"])
            for j in range(8):
                P.op("pe", lambda e, j=j: e.matmul(psh(1), lhsT=onesD[:], rhs=sq[:, j, :], start=(j == 0), stop=(j == 7)),
                     reads=["onesD", sq_names[j // 2]], writes=["ps1"])
            P.op("act", lambda e: e.copy(out=mS[:], in_=psh(0)), reads=["ps0"], writes=["mS"])
            P.op("dve", lambda e: e.tensor_tensor(out=vS[:], in0=mS[:], in1=psh(0), op=ALU.mult), reads=["mS", "ps0"], writes=["vS"])
            P.op("dve", lambda e: e.scalar_tensor_tensor(out=vS[:], in0=vS[:], scalar=-1.0, in1=psh(1), op0=ALU.mult, op1=ALU.add), reads=["vS", "ps1"], writes=["vS"])
            P.op("act", lambda e: e.activation(out=rS[:], in_=vS[:], func=AF.Sqrt, bias=eps), reads=["vS"], writes=["rS"])
            P.op("dve", lambda e: e.reciprocal(out=rS[:], in_=rS[:]), reads=["rS"], writes=["rS"])
            for b in range(4):
                P.op("pool", lambda e, b=b: e.tensor_tensor(out=res[:, 2 * b:2 * b + 2, :], in0=res[:, 2 * b:2 * b + 2, :],
                     in1=mS[:].unsqueeze(1).to_broadcast([128, 2, T]), op=ALU.subtract), reads=[res_names[b], "mS"], writes=[res_names[b]])
                P.op("pool", lambda e, b=b: e.tensor_tensor(out=res[:, 2 * b:2 * b + 2, :], in0=res[:, 2 * b:2 * b + 2, :],
                     in1=rS[:].unsqueeze(1).to_broadcast([128, 2, T]), op=ALU.mult), reads=[res_names[b], "rS"], writes=[res_names[b]])

        def affine_fm(src, src_names, tmp, tmp_names, dst, dst_name, g8, b8, pres):
            P.op("dve", lambda e: e.tensor_tensor(out=tmp, in0=src, in1=bc_t(g8), op=ALU.mult), reads=src_names + pres, writes=tmp_names)
            P.op("pool", lambda e: e.tensor_tensor(out=dst, in0=tmp, in1=bc_t(b8), op=ALU.add), reads=tmp_names + pres, writes=[dst_name])

        def dump(name, ap, res):
            if name in dbg_d:
                P.op("sp", lambda e: e.dma_start(out=dbg_d[name], in_=ap), reads=res, dma_sem="dbg")

        FA, FB, FC, FD = Fv
        FA4 = scr[:, 0:2048].rearrange("p (m s t) -> p m s t", m=8, s=2)

        for s in (range(nseq) if "C" in phases else []):
            P.op("pool", lambda e: e.memset(convh[:], 0.0), reads=["convh"], writes=["convh"])
            P.op("pool", lambda e: e.memset(hst[:], 0.0), reads=["hst"], writes=["hst"])
            for it in range(nt):
                t0 = it * T
                P.op("sp", lambda e, s=s, t0=t0: e.dma_start(out=xtok[:], in_=x_d[s, t0:t0 + T, :].rearrange("(u p) d -> p u d", p=128)),
                     writes=["vg0", "vg1", "vg2", "vg3"], dma_sem="xl")
                for m in range(8):
                    for u in range(2):
                        P.op("pe", lambda e, m=m, u=u: e.transpose(out=psh(m)[:, u * 128:(u + 1) * 128], in_=xtok[:, u, m * 128:(m + 1) * 128], identity=ident[:]),
                             reads=["vg%d" % (u * 2 + m // 4), "ident"], writes=["ps%d" % m])
                for b in range(4):
                    P.op("act", lambda e, b=b: e.copy(out=xT[:, 2 * b:2 * b + 2, :], in_=psb(b).rearrange("p (m t) -> p m t", t=T)),
                         reads=psn(2 * b, 2 * b + 2), writes=["xT"])
                for l in range(LD):
                    d = lambda k, l=l, s=s: dv[:, l, s, k, :]
                    affine_fm(xT[:], ["xT"], FD, fres(3), hT[:], "hT", d(0), d(1), ["dv"])
                    for half in range(2):
                        slot = wload(wmix_d[l][0 * 2 + half], 4096)
                        wv = ring[:, slot, 0:4096].rearrange("p (j n) -> p j n", n=512)
                        for u in range(2):
                            for j in range(8):
                                P.op("pe", lambda e, wv=wv, u=u, j=j, half=half: e.matmul(
                                    psb(u * 2 + half), lhsT=hT[:, j, u * 128:(u + 1) * 128], rhs=wv[:, j, :], start=(j == 0), stop=(j == 7)),
                                    reads=["ring%d" % slot, "hT"], writes=psn(2 * (u * 2 + half), 2 * (u * 2 + half) + 2))
                    for u in range(2):
                        for half in range(2):
                            b = u * 2 + half
                            gelu2(psb(b), xtok[:, u, half * 512:(half + 1) * 512], psn(2 * b, 2 * b + 2), ["vg%d" % b])
                            P.op("dve", lambda e, u=u, half=half: e.bn_stats(out=st6[:, u, half, :], in_=xtok[:, u, half * 512:(half + 1) * 512]),
                                 reads=["vg%d" % b], writes=["st6_%d" % b])
                        P.op("dve", lambda e, u=u: e.bn_aggr(out=mv[:, u, :], in_=st6[:, u, :, :].rearrange("p a b -> p (a b)")),
                             reads=["st6_%d" % (u * 2), "st6_%d" % (u * 2 + 1)], writes=["mv%d" % u])
                    P.op("act", lambda e: e.activation(out=rstd[:], in_=mv[:, :, 1], func=AF.Sqrt, bias=4.0 * EPS), reads=["mv0", "mv1"], writes=["rstd"])
                    P.op("dve", lambda e: e.reciprocal(out=rstd[:], in_=rstd[:]), reads=["rstd"], writes=["rstd"])
                    for u in range(2):
                        P.op("dve", lambda e, u=u: e.tensor_scalar(out=vn[:, u, :], in0=xtok[:, u, :], scalar1=mv[:, u, 0:1], scalar2=rstd[:, u:u + 1],
                             op0=ALU.subtract, op1=ALU.mult), reads=["vg%d" % (2 * u), "vg%d" % (2 * u + 1), "mv%d" % u, "rstd"], writes=["BB%d" % (2 * u), "BB%d" % (2 * u + 1)])
                    for g in range(8):
                        for u in range(2):
                            P.op("pe", lambda e, g=g, u=u, l=l: e.matmul(psh(8 + g)[:, u * 128:(u + 1) * 128], lhsT=vn[:, u, g * 128:(g + 1) * 128],
                                 rhs=WsT[:, l, g, :], start=True, stop=True), reads=["BB%d" % (2 * u + g // 4), "WsT"], writes=["ps%d" % (8 + g)])
                        P.op("dve", lambda e, g=g, l=l: e.scalar_tensor_tensor(
                            out=FA4[:, g, :, :], in0=psh(8 + g).rearrange("p (s t) -> p s t", s=2), scalar=pv("sgu_ln_g", l)[:, g:g + 1],
                            in1=bias2[:, l, g, :].unsqueeze(1).to_broadcast([128, 2, 128]), op0=ALU.mult, op1=ALU.add),
                            reads=["ps%d" % (8 + g), "prm", "bias2"], writes=["FA%d" % (g // 2)])
                    fm_matmul(l, 1, hT, ["hT"], 0)
                    for b in range(4):
                        gelu2(psb(b), FB[:, 2 * b:2 * b + 2, :].rearrange("p m t -> p (m t)"), psn(2 * b, 2 * b + 2), ["FB%d" % b])
                        P.op("pool", lambda e, b=b: e.tensor_tensor(out=BA[:, 2 * b:2 * b + 2, :], in0=FB[:, 2 * b:2 * b + 2, :], in1=FA[:, 2 * b:2 * b + 2, :], op=ALU.mult),
                             reads=["FB%d" % b, "FA%d" % b], writes=["BA%d" % b])
                    fm_matmul(l, 2, hT, ["hT"], 8)
                    P.op("pool", lambda e, l=l: e.tensor_copy(out=rin[:, :, 0:3], in_=convh[:, l, :, :]), reads=["convh"], writes=["rinh"])
                    for b in range(4):
                        P.op("act", lambda e, b=b: e.copy(out=rin[:, 2 * b:2 * b + 2, 3:3 + T], in_=psb(4 + b).rearrange("p (m t) -> p m t", t=T)),
                             reads=psn(8 + 2 * b, 10 + 2 * b), writes=["rin%d" % b])
                    rin_all = ["rinh"] + ["rin%d" % b for b in range(4)]
                    P.op("pool", lambda e, l=l: e.tensor_copy(out=convh[:, l, :, :], in_=rin[:, :, T:T + 3]), reads=rin_all + ["convh"], writes=["convh"])
                    P.op("pool", lambda e, l=l: e.tensor_tensor(out=FB, in0=rin[:, :, 0:T], in1=bc_t(pv("cw0", l)), op=ALU.mult), reads=rin_all + ["prm"], writes=fres(1))
                    P.op("pool", lambda e, l=l: e.tensor_tensor(out=FB, in0=FB, in1=bc_t(pv("conv_b", l)), op=ALU.add), reads=fres(1) + ["prm"], writes=fres(1))
                    for k in range(1, 4):
                        P.op("pool", lambda e, l=l, k=k: e.tensor_tensor(out=FC, in0=rin[:, :, k:k + T], in1=bc_t(pv("cw%d" % k, l)), op=ALU.mult), reads=rin_all + ["prm"], writes=fres(2))
                        P.op("pool", lambda e: e.tensor_tensor(out=FB, in0=FB, in1=FC, op=ALU.add), reads=fres(1) + fres(2), writes=fres(1))
                    P.op("act", lambda e: e.copy(out=BB[:], in_=FB), reads=fres(1), writes=["BB%d" % b for b in range(4)])
                    for wi in range(2):
                        for h in range(8):
                            P.op("pe", lambda e, l=l, wi=wi, h=h: e.matmul(psh(wi * 8 + h), lhsT=rgw[:, l, wi, h, :], rhs=BB[:, h, :], start=True, stop=True),
                                 reads=["rgw", "BB%d" % (h // 2)], writes=["ps%d" % (wi * 8 + h)])
                    for h in range(8):
                        P.op("act", lambda e, l=l, h=h: e.activation(out=FC[:, h, :], in_=psh(h), func=AF.Tanh, scale=0.5, bias=spm[:, l, 2, h:h + 1]),
                             reads=["ps%d" % h, "spm"], writes=["FC%d" % (h // 2)])
                        P.op("act", lambda e, l=l, h=h: e.activation(out=FD[:, h, :], in_=FC[:, h, :], func=AF.Exp, scale=spm[:, l, 0, h:h + 1], bias=spm[:, l, 0, h:h + 1]),
                             reads=["FC%d" % (h // 2), "spm"], writes=["FD%d" % (h // 2)])
                        P.op("act", lambda e, l=l, h=h: e.activation(out=FC[:, h, :], in_=FC[:, h, :], func=AF.Exp, scale=spm[:, l, 1, h:h + 1], bias=spm[:, l, 1, h:h + 1]),
                             reads=["FC%d" % (h // 2), "spm"], writes=["FC%d" % (h // 2)])
                        P.op("act", lambda e, l=l, h=h: e.activation(out=FA[:, h, :], in_=psh(8 + h), func=AF.Tanh, scale=0.5, bias=spm[:, l, 3, h:h + 1]),
                             reads=["ps%d" % (8 + h), "spm"], writes=["FA%d" % (h // 2)])
                    P.op("pool", lambda e: e.tensor_scalar(out=FC, in0=FC, scalar1=-1.0, scalar2=1.0, op0=ALU.mult, op1=ALU.add), reads=fres(2), writes=fres(2))
                    P.op("act", lambda e: e.activation(out=FC, in_=FC, func=AF.Sqrt), reads=fres(2), writes=fres(2))
                    P.op("dve", lambda e: e.scalar_tensor_tensor(out=FA, in0=FA, scalar=1.0, in1=FB, op0=ALU.add, op1=ALU.mult), reads=fres(0) + fres(1), writes=fres(0))
                    P.op("pool", lambda e: e.tensor_tensor(out=FC, in0=FC, in1=FA, op=ALU.mult), reads=fres(2) + fres(0), writes=fres(2))
                    for h in range(8):
                        P.op("dve", lambda e, l=l, h=h: e.tensor_tensor_scan(out=FA[:, h, :], data0=FD[:, h, :], data1=FC[:, h, :], initial=hst[:, l, h:h + 1], op0=ALU.mult, op1=ALU.add),
                             reads=["FD%d" % (h // 2), "FC%d" % (h // 2), "hst", "FA%d" % (h // 2)], writes=["FA%d" % (h // 2)])
                    P.op("pool", lambda e, l=l: e.tensor_copy(out=hst[:, l, :], in_=FA[:, :, T - 1]), reads=fres(0) + ["hst"], writes=["hst"])
                    fm_matmul(l, 3, hT, ["hT"], 0)
                    for b in range(4):
                        gelu2(psb(b), FB[:, 2 * b:2 * b + 2, :].rearrange("p m t -> p (m t)"), psn(2 * b, 2 * b + 2), ["FB%d" % b])
                        P.op("pool", lambda e, b=b: e.tensor_tensor(out=BB[:, 2 * b:2 * b + 2, :], in0=FB[:, 2 * b:2 * b + 2, :], in1=FA[:, 2 * b:2 * b + 2, :], op=ALU.mult),
                             reads=["FB%d" % b, "FA%d" % b], writes=["BB%d" % b])
                    fm_matmul(l, 4, hT, ["hT"], 8)
                    for b in range(4):
                        P.op("act", lambda e, b=b: e.activation(out=FC[:, 2 * b:2 * b + 2, :], in_=psb(4 + b).rearrange("p (m t) -> p m t", t=T), func=AF.Tanh, scale=0.5),
                             reads=psn(8 + 2 * b, 10 + 2 * b), writes=["FC%d" % b])
                    fm_matmul(l, 5, hT, ["hT"], 0)
                    for b in range(4):
                        P.op("act", lambda e, b=b: e.activation(out=FD[:, 2 * b:2 * b + 2, :], in_=psb(b).rearrange("p (m t) -> p m t", t=T), func=AF.Tanh, scale=0.5),
                             reads=psn(2 * b, 2 * b + 2), writes=["FD%d" % b])
                    fm_matmul(l, 6, BA, ["BA%d" % b for b in range(4)], 8)
                    for b in range(4):
                        P.op("dve", lambda e, b=b: e.scalar_tensor_tensor(out=FC[:, 2 * b:2 * b + 2, :], in0=FC[:, 2 * b:2 * b + 2, :], scalar=1.0,
                             in1=psb(4 + b).rearrange("p (m t) -> p m t", t=T), op0=ALU.add, op1=ALU.mult), reads=["FC%d" % b] + psn(8 + 2 * b, 10 + 2 * b), writes=["FC%d" % b])
                    fm_matmul(l, 7, BB, ["BB%d" % b for b in range(4)], 0)
                    for b in range(4):
                        P.op("dve", lambda e, b=b: e.scalar_tensor_tensor(out=FD[:, 2 * b:2 * b + 2, :], in0=FD[:, 2 * b:2 * b + 2, :], scalar=1.0,
                             in1=psb(b).rearrange("p (m t) -> p m t", t=T), op0=ALU.add, op1=ALU.mult), reads=["FD%d" % b] + psn(2 * b, 2 * b + 2), writes=["FD%d" % b])
                        P.op("dve", lambda e, b=b: e.scalar_tensor_tensor(out=BA[:, 2 * b:2 * b + 2, :], in0=FC[:, 2 * b:2 * b + 2, :], scalar=2.0,
                             in1=FD[:, 2 * b:2 * b + 2, :], op0=ALU.mult, op1=ALU.add), reads=["FC%d" % b, "FD%d" % b], writes=["BA%d" % b])
                    fm_matmul(l, 8, BA, ["BA%d" % b for b in range(4)], 8)
                    for m in range(8):
                        P.op("dve", lambda e, m=m, d=d: e.scalar_tensor_tensor(out=FC[:, m, :], in0=psh(8 + m), scalar=d(2)[:, m:m + 1], in1=xT[:, m, :], op0=ALU.mult, op1=ALU.add),
                             reads=["ps%d" % (8 + m), "dv", "xT"], writes=["FC%d" % (m // 2)])
                    layer_norm_fm(FC, fres(2), FD, fres(3), EPS / (ALPHA * ALPHA))
                    affine_fm(FC, fres(2), FD, fres(3), xT[:], "xT", pv("ln1_g", l), pv("ln1_b", l), ["prm"])
                    affine_fm(FC, fres(2), FA, fres(0), hT[:], "hT", d(3), d(4), ["dv"])
                    if l == 0 and it == 0 and s == 0:
                        dump("x1", xT[:], ["xT"])
                    for u in range(2):
                        for j in range(8):
                            P.op("pe", lambda e, u=u, j=j, l=l: e.matmul(psh(2)[:, u * NE:(u + 1) * NE], lhsT=hT[:, j, u * 128:(u + 1) * 128], rhs=wr[:, l, j, :], start=(j == 0), stop=(j == 7)),
                                 reads=["hT", "wr"], writes=["ps2"])
                    lg = psh(2)[:, 0:2 * NE].rearrange("p (u n) -> p u n", u=2)
                    P.op("act", lambda e: e.activation(out=r_sc[:], in_=lg, func=AF.Tanh, scale=0.5), reads=["ps2"], writes=["r_sc"])
                    P.op("dve", lambda e: e.tensor_scalar(out=r_sc[:], in0=r_sc[:], scalar1=0.5, scalar2=0.5, op0=ALU.mult, op1=ALU.add), reads=["r_sc"], writes=["r_sc"])
                    P.op("dve", lambda e, l=l: e.tensor_tensor(out=r_bi[:], in0=r_sc[:], in1=rb[:, l, :].unsqueeze(1).to_broadcast([128, 2, NE]), op=ALU.add), reads=["r_sc", "rb"], writes=["r_bi"])
                    bi4 = r_bi[:].rearrange("p u (g k) -> p u g k", k=8)
                    P.op("dve", lambda e: e.tensor_reduce(out=r_m1[:], in_=bi4, axis=AX.X, op=ALU.max), reads=["r_bi"], writes=["r_m1"])
                    t4 = r_t[:].rearrange("p u (g k) -> p u g k", k=8)
                    P.op("dve", lambda e: e.tensor_tensor(out=t4, in0=bi4, in1=r_m1[:].unsqueeze(3).to_broadcast([128, 2, 8, 8]), op=ALU.is_ge), reads=["r_bi", "r_m1"], writes=["r_t"])
                    P.op("dve", lambda e: e.scalar_tensor_tensor(out=r_t[:], in0=r_t[:], scalar=-1e9, in1=r_bi[:], op0=ALU.mult, op1=ALU.add), reads=["r_t", "r_bi"], writes=["r_t"])
                    P.op("dve", lambda e: e.tensor_reduce(out=r_m2[:], in_=t4, axis=AX.X, op=ALU.max), reads=["r_t"], writes=["r_m2"])
                    P.op("dve", lambda e: e.tensor_tensor(out=r_m1[:], in0=r_m1[:], in1=r_m2[:], op=ALU.add), reads=["r_m1", "r_m2"], writes=["r_m1"])
                    for u in range(2):
                        P.op("dve", lambda e, u=u: e.max(out=r_s8[:, u, :], in_=r_m1[:, u, :]), reads=["r_m1"], writes=["r_s8"])
                    for u in range(2):
                        P.op("dve", lambda e, u=u: e.tensor_scalar(out=r_k[:, u, :], in0=r_m1[:, u, :], scalar1=r_s8[:, u, 3:4], scalar2=None, op0=ALU.is_ge), reads=["r_m1", "r_s8"], writes=["r_k"])
                    kb = r_k[:].unsqueeze(3).to_broadcast([128, 2, 8, 8])
                    mb4 = r_mb[:].rearrange("p u (g k) -> p u g k", k=8)
                    P.op("dve", lambda e: e.tensor_tensor(out=mb4, in0=bi4, in1=kb, op=ALU.mult), reads=["r_bi", "r_k"], writes=["r_mb"])
                    P.op("dve", lambda e: e.tensor_scalar(out=r_k[:], in0=r_k[:], scalar1=1e9, scalar2=-1e9, op0=ALU.mult, op1=ALU.add), reads=["r_k", "r_mb"], writes=["r_k"])
                    P.op("dve", lambda e: e.tensor_tensor(out=mb4, in0=mb4, in1=kb, op=ALU.add), reads=["r_mb", "r_k"], writes=["r_mb"])
                    for u in range(2):
                        P.op("dve", lambda e, u=u: e.max(out=r_e8[:, u, :], in_=r_mb[:, u, :]), reads=["r_mb"], writes=["r_e8"])
                    for u in range(2):
                        P.op("dve", lambda e, u=u: e.tensor_scalar(out=r_w[:, u, :], in0=r_mb[:, u, :], scalar1=r_e8[:, u, 7:8], scalar2=None, op0=ALU.is_ge), reads=["r_mb", "r_e8"], writes=["r_w"])
                    P.op("dve", lambda e: e.tensor_tensor(out=r_w[:], in0=r_w[:], in1=r_sc[:], op=ALU.mult), reads=["r_w", "r_sc"], writes=["r_w"])
                    P.op("dve", lambda e: e.tensor_reduce(out=r_d[:], in_=r_w[:], axis=AX.X, op=ALU.add), reads=["r_w"], writes=["r_d"])
                    P.op("dve", lambda e: e.reciprocal(out=r_d[:], in_=r_d[:]), reads=["r_d"], writes=["r_d"])
                    for u in range(2):
                        P.op("dve", lambda e, u=u: e.tensor_scalar(out=r_G[:, u, :], in0=r_w[:, u, :], scalar1=r_d[:, u:u + 1], scalar2=1.25, op0=ALU.mult, op1=ALU.mult), reads=["r_w", "r_d"], writes=["r_G"])
                    slots = {}
                    NU = 2 * (NE + 1)

                    def up(n):
                        ex, u = n // 2, n % 2
                        if u == 0:
                            slots[ex] = wload(wexp_d[l][ex], 6144)
                        slot = slots[ex]
                        p = n % 2
                        w13 = ring[:, slot, 0:4096].rearrange("p (j n) -> p j n", n=512)
                        for j in range(8):
                            P.op("pe", lambda e, w13=w13, j=j, u=u, p=p: e.matmul(psb(p), lhsT=hT[:, j, u * 128:(u + 1) * 128], rhs=w13[:, j, :], start=(j == 0), stop=(j == 7)),
                                 reads=["ring%d" % slot, "hT"], writes=["pb%d" % p])
                        h1 = psb(p)[:, 0:256]
                        h3 = psb(p)[:, 256:512]
                        P.op("act", lambda e: e.activation(out=tbq[p][:], in_=h1, func=AF.Tanh, scale=0.5), reads=["pb%d" % p], writes=["tb%d" % p])
                        P.op("dve", lambda e: e.scalar_tensor_tensor(out=tbq[p][:], in0=tbq[p][:], scalar=1.0, in1=h1, op0=ALU.add, op1=ALU.mult), reads=["tb%d" % p, "pb%d" % p], writes=["tb%d" % p])
                        if ex < NE:
                            P.op("dve", lambda e: e.scalar_tensor_tensor(out=htok[p][:], in0=tbq[p][:], scalar=r_G[:, u, ex:ex + 1], in1=h3, op0=ALU.mult, op1=ALU.mult),
                                 reads=["tb%d" % p, "pb%d" % p, "r_G"], writes=["ht%d" % p])
                        else:
                            P.op("dve", lambda e: e.scalar_tensor_tensor(out=htok[p][:], in0=tbq[p][:], scalar=0.5, in1=h3, op0=ALU.mult, op1=ALU.mult),
                                 reads=["tb%d" % p, "pb%d" % p], writes=["ht%d" % p])

                    def tp(n):
                        p = n % 2
                        q = n % 3
                        for fc in range(2):
                            P.op("pe", lambda e, fc=fc: e.transpose(out=tpv[p][:, fc * 128:(fc + 1) * 128], in_=htok[p][:, fc * 128:(fc + 1) * 128], identity=identb[:]),
                                 reads=["ht%d" % p, "identb"], writes=["pb%d" % (2 + p)])
                        P.op("act", lambda e: e.copy(out=hx[q][:], in_=tpv[p][:, 0:256].rearrange("p (f t) -> p f t", f=2)), reads=["pb%d" % (2 + p)], writes=["hx%d" % q])

                    def down(n):
                        ex, u = n // 2, n % 2
                        q = n % 3
                        slot = slots[ex]
                        w2 = ring[:, slot, 4096:6144].rearrange("p (f o) -> p f o", f=2)
                        for half in range(2):
                            for fc in range(2):
                                P.op("pe", lambda e, w2=w2, half=half, fc=fc: e.matmul(psb(4 + u * 2 + half), lhsT=hx[q][:, fc, :], rhs=w2[:, fc, half * 512:(half + 1) * 512],
                                     start=(ex == 0 and fc == 0), stop=(ex == NE and fc == 1)),
                                     reads=["ring%d" % slot, "hx%d" % q], writes=["pb%d" % (4 + u * 2 + half)])
                    for n in range(NU + 2):
                        if n < NU:
                            up(n)
                        if 1 <= n <= NU:
                            tp(n - 1)
                        if n >= 2:
                            down(n - 2)
                    for u in range(2):
                        for half in range(2):
                            b = u * 2 + half
                            if half == 0:
                                P.op("act", lambda e, u=u, half=half, b=b: e.copy(out=xtok[:, u, half * 512:(half + 1) * 512], in_=psb(4 + b)), reads=["pb%d" % (4 + b)], writes=["vg%d" % b])
                            else:
                                P.op("dve", lambda e, u=u, half=half, b=b: e.tensor_copy(out=xtok[:, u, half * 512:(half + 1) * 512], in_=psb(4 + b)), reads=["pb%d" % (4 + b)], writes=["vg%d" % b])
                    for m in range(8):
                        for u in range(2):
                            P.op("pe", lambda e, m=m, u=u: e.transpose(out=psh(m)[:, u * 128:(u + 1) * 128], in_=xtok[:, u, m * 128:(m + 1) * 128], identity=ident[:]),
                                 reads=["vg%d" % (u * 2 + m // 4), "ident"], writes=["ps%d" % m])
                    for m in range(8):
                        P.op("dve", lambda e, m=m, d=d: e.scalar_tensor_tensor(out=FC[:, m, :], in0=psh(m), scalar=d(5)[:, m:m + 1], in1=xT[:, m, :], op0=ALU.mult, op1=ALU.add),
                             reads=["ps%d" % m, "dv", "xT"], writes=["FC%d" % (m // 2)])
                    layer_norm_fm(FC, fres(2), FD, fres(3), EPS / (ALPHA * ALPHA))
                    affine_fm(FC, fres(2), FD, fres(3), xT[:], "xT", pv("ln2_g", l), pv("ln2_b", l), ["prm"])
                    if l == 0 and it == 0 and s == 0:
                        dump("x2", xT[:], ["xT"])
                for u in range(2):
                    for m in range(8):
                        P.op("pe", lambda e, u=u, m=m: e.transpose(out=psb(u * 2 + m // 4)[:, (m % 4) * 128:(m % 4 + 1) * 128], in_=xT[:, m, u * 128:(u + 1) * 128], identity=ident[:]),
                             reads=["xT", "ident"], writes=psn(2 * (u * 2 + m // 4), 2 * (u * 2 + m // 4) + 2))
                    for half in range(2):
                        b = u * 2 + half
                        if half == 0:
                            P.op("act", lambda e, u=u, half=half, b=b: e.copy(out=xtok[:, u, half * 512:(half + 1) * 512], in_=psb(b)), reads=psn(2 * b, 2 * b + 2) + ["vg%d" % b], writes=["vg%d" % b])
                        else:
                            P.op("dve", lambda e, u=u, half=half, b=b: e.tensor_copy(out=xtok[:, u, half * 512:(half + 1) * 512], in_=psb(b)), reads=psn(2 * b, 2 * b + 2) + ["vg%d" % b], writes=["vg%d" % b])
                P.op("sp", lambda e, s=s, t0=t0: e.dma_start(out=out_d[s, t0:t0 + T, :].rearrange("(u p) d -> p u d", p=128), in_=xtok[:]),
                     reads=["vg%d" % b for b in range(4)], dma_sem="st")

        P.finalize()
        with nc.Block() as block:
            @block.sync
            def _(e):
                P.emit("sp", e, sems)
                for s_, v_ in P.dma_sem_count.items():
                    if s_ in ("st", "dbg"):
                        e.wait_ge(sems[s_], v_)

            @block.scalar
            def _(e):
                P.emit("act", e, sems)

            @block.vector
            def _(e):
                P.emit("dve", e, sems)

            @block.gpsimd
            def _(e):
                P.emit("pool", e, sems)

            @block.tensor
            def _(e):
                P.emit("pe", e, sems)
    return nc, P


def kernel(**inputs):
    ncores = 8
    x = np.ascontiguousarray(inputs["x"], dtype=np.float32)
    c = np.ascontiguousarray(inputs["c"], dtype=np.float32)
    nc, _ = build_program(nseq=2, nt=16, depth=4)
    wmap = {n: np.ascontiguousarray(inputs[n], dtype=np.float32) for n in WNAMES}
    in_maps = []
    for i in range(ncores):
        m = dict(wmap)
        m["x"] = x[2 * i:2 * i + 2]
        m["c"] = c[2 * i:2 * i + 2]
        in_maps.append(m)
    res = run_bass_kernel_spmd(nc, in_maps, core_ids=list(range(ncores)))
    return np.concatenate([r["out"] for r in res.results], axis=0)
```
